# Optimizing a Trainium2 kernel written in Bass

```python
import jax, jax.numpy as jnp
from jax import lax
import numpy as np

D_MODEL = 1024
BATCH = 16
SEQ = 2048
DEPTH = 1

MEM_LEN = 256
GLA_H = 4
GLA_QK = D_MODEL // 2
GLA_V = D_MODEL
GLA_DK = GLA_QK // GLA_H
GLA_DV = GLA_V // GLA_H
GLA_RANK = 16
GLA_TAU = 16.0
GLA_CHUNK = 64
SWA_HD = 64
SWA_HQ = D_MODEL // SWA_HD
SWA_HKV = 4
SWA_Q = SWA_HQ * SWA_HD
SWA_KV = SWA_HKV * SWA_HD
WINDOW = 128
ROPE_THETA = 10000.0
X_H = 4
X_HD = 256
X_W = X_H * X_HD
N_BRANCH = 3
IN_SIZES = (GLA_QK, GLA_QK, GLA_V, GLA_V, GLA_RANK,
            SWA_Q, SWA_KV, SWA_KV, SWA_Q,
            X_W, X_W,
            N_BRANCH * D_MODEL)
IN_COLS = sum(IN_SIZES)
EPS = 1e-6

kernel_name = "hybrid_gla_swa_sink_memxattn_gated"


def rmsnorm(x, g):
    xf = x.astype(jnp.float32)
    y = xf * lax.rsqrt(jnp.mean(xf * xf, axis=-1, keepdims=True) + EPS)
    return (y * g.astype(jnp.float32)).astype(x.dtype)


def rope(x, pos):
    half = x.shape[-1] // 2
    inv = ROPE_THETA ** (-jnp.arange(half, dtype=jnp.float32) / half)
    ang = pos.astype(jnp.float32)[:, None] * inv[None, :]
    cos = jnp.cos(ang)[None, :, None, :]
    sin = jnp.sin(ang)[None, :, None, :]
    xf = x.astype(jnp.float32)
    x1, x2 = xf[..., :half], xf[..., half:]
    out = jnp.concatenate([x1 * cos - x2 * sin, x2 * cos + x1 * sin], axis=-1)
    return out.astype(x.dtype)


def gla_chunked(q, k, v, log_a):
    B, S, H, DK = q.shape
    DV = v.shape[-1]
    C = GLA_CHUNK
    N = S // C

    def blk(t):
        return t.astype(jnp.float32).reshape(B, N, C, H, t.shape[-1]).transpose(0, 3, 1, 2, 4)

    qf = blk(q) * (DK ** -0.5)
    kf = blk(k)
    vf = blk(v)
    b = jnp.cumsum(blk(log_a), axis=3)
    b_last = b[:, :, :, -1:, :]
    q_dec = qf * jnp.exp(b)
    att = jnp.einsum('bhncd,bhnjd->bhncj', q_dec, kf * jnp.exp(-b))
    causal = jnp.tril(jnp.ones((C, C), dtype=bool))
    att = jnp.where(causal, att, 0.0)
    o_intra = jnp.einsum('bhncj,bhnjv->bhncv', att, vf)
    k_dec = kf * jnp.exp(b_last - b)
    chunk_decay = jnp.exp(b_last[:, :, :, 0, :])

    def step(state, inp):
        q_c, k_c, v_c, d_c = inp
        o_c = jnp.einsum('bhcd,bhdv->bhcv', q_c, state)
        state = d_c[..., None] * state + jnp.einsum('bhcd,bhcv->bhdv', k_c, v_c)
        return state, o_c

    xs = (jnp.moveaxis(q_dec, 2, 0), jnp.moveaxis(k_dec, 2, 0),
          jnp.moveaxis(vf, 2, 0), jnp.moveaxis(chunk_decay, 2, 0))
    _, o_inter = lax.scan(step, jnp.zeros((B, H, DK, DV), jnp.float32), xs)
    o = o_intra + jnp.moveaxis(o_inter, 0, 2)
    return o.transpose(0, 2, 3, 1, 4).reshape(B, S, H, DV)


def swa_sinks(q, k, v, sinks):
    B, S, HQ, D = q.shape
    HKV = k.shape[2]
    G = HQ // HKV
    W = WINDOW
    N = S // W
    qb = q.astype(jnp.float32).reshape(B, N, W, HKV, G, D)
    kb = k.astype(jnp.float32).reshape(B, N, W, HKV, D)
    vb = v.astype(jnp.float32).reshape(B, N, W, HKV, D)
    pad = ((0, 0), (1, 0), (0, 0), (0, 0), (0, 0))
    kk = jnp.concatenate([jnp.pad(kb, pad)[:, :N], kb], axis=2)
    vv = jnp.concatenate([jnp.pad(vb, pad)[:, :N], vb], axis=2)
    s = jnp.einsum('bnqhgd,bnkhd->bnhgqk', qb, kk) * (D ** -0.5)
    qi = jnp.arange(W)[:, None] + W
    kj = jnp.arange(2 * W)[None, :]
    band = (kj <= qi) & (kj > qi - W)
    has_prev = (jnp.arange(N) > 0)[:, None, None]
    valid = band[None] & (has_prev | (kj >= W)[None])
    s = jnp.where(valid[None, :, None, None], s, -jnp.inf)
    sink = sinks.astype(jnp.float32).reshape(1, 1, HKV, G, 1, 1)
    m = jnp.maximum(jnp.max(s, axis=-1, keepdims=True), sink)
    p = jnp.exp(s - m)
    p = p / (jnp.sum(p, axis=-1, keepdims=True) + jnp.exp(sink - m))
    o = jnp.einsum('bnhgqk,bnkhd->bnqhgd', p, vv)
    return o.reshape(B, S, HQ, D)


def setup_inputs(seed: int = 0) -> dict:
    key = jax.random.key(seed)
    ks = jax.random.split(key, 16)
    L = DEPTH

    def nrm(k, shape, scale):
        return jax.random.normal(k, shape, jnp.float32) * scale

    return {
        "x": nrm(ks[0], (BATCH, SEQ, D_MODEL), 1.0),
        "mem": nrm(ks[1], (BATCH, MEM_LEN, D_MODEL), 1.0),
        "g_mix": 1.0 + nrm(ks[2], (L, D_MODEL), 0.02),
        "g_mem": 1.0 + nrm(ks[3], (L, D_MODEL), 0.02),
        "w_in": nrm(ks[4], (L, D_MODEL, IN_COLS), D_MODEL ** -0.5),
        "b_in": nrm(ks[5], (L, IN_COLS), 0.01),
        "w_gla_gate_up": nrm(ks[6], (L, GLA_RANK, GLA_QK), GLA_RANK ** -0.5),
        "b_gla_gate": nrm(ks[7], (L, GLA_QK), 0.1),
        "g_gla_norm": 1.0 + nrm(ks[8], (L, GLA_DV), 0.02),
        "sinks": nrm(ks[9], (L, SWA_HQ), 0.5),
        "w_mem_kv": nrm(ks[10], (L, D_MODEL, 2 * X_W), D_MODEL ** -0.5),
        "w_br_gla": nrm(ks[11], (L, GLA_V, D_MODEL), GLA_V ** -0.5),
        "w_br_swa": nrm(ks[12], (L, SWA_Q, D_MODEL), SWA_Q ** -0.5),
        "w_br_mem": nrm(ks[13], (L, X_W, D_MODEL), X_W ** -0.5),
        "w_out": nrm(ks[14], (L, D_MODEL, D_MODEL), D_MODEL ** -0.5),
        "g_final": 1.0 + nrm(ks[15], (D_MODEL,), 0.02),
    }


def reference(x, mem, g_mix, g_mem, w_in, b_in, w_gla_gate_up, b_gla_gate, g_gla_norm, sinks,
              w_mem_kv, w_br_gla, w_br_swa, w_br_mem, w_out, g_final):
    B, S, _ = x.shape
    M = mem.shape[1]
    dt = x.dtype
    pos = jnp.arange(S)
    split_idx = tuple(int(i) for i in np.cumsum(IN_SIZES)[:-1])
    for l in range(DEPTH):
        h = rmsnorm(x, g_mix[l])
        proj = h @ w_in[l] + b_in[l]
        (gq, gk, gv, gz, glr, sq, sk, sv, sz, xq, xz, gates) = jnp.split(proj, split_idx, axis=-1)

        log_a = jax.nn.log_sigmoid((glr @ w_gla_gate_up[l] + b_gla_gate[l]).astype(jnp.float32)) / GLA_TAU
        o_a = gla_chunked(gq.reshape(B, S, GLA_H, GLA_DK), gk.reshape(B, S, GLA_H, GLA_DK),
                          gv.reshape(B, S, GLA_H, GLA_DV), log_a.reshape(B, S, GLA_H, GLA_DK))
        o_a = rmsnorm(o_a, g_gla_norm[l]).reshape(B, S, GLA_V).astype(dt)
        y_a = (o_a * jax.nn.silu(gz)) @ w_br_gla[l]

        q_b = rope(sq.reshape(B, S, SWA_HQ, SWA_HD), pos)
        k_b = rope(sk.reshape(B, S, SWA_HKV, SWA_HD), pos)
        o_b = swa_sinks(q_b, k_b, sv.reshape(B, S, SWA_HKV, SWA_HD), sinks[l])
        y_b = (o_b.reshape(B, S, SWA_Q).astype(dt) * jax.nn.silu(sz)) @ w_br_swa[l]

        mkv = rmsnorm(mem, g_mem[l]) @ w_mem_kv[l]
        mk = mkv[..., :X_W].reshape(B, M, X_H, X_HD).astype(jnp.float32)
        mv = mkv[..., X_W:].reshape(B, M, X_H, X_HD).astype(jnp.float32)
        s_c = jnp.einsum('bshd,bmhd->bhsm', xq.reshape(B, S, X_H, X_HD).astype(jnp.float32), mk) * (X_HD ** -0.5)
        p_c = jax.nn.softmax(s_c, axis=-1)
        o_c = jnp.einsum('bhsm,bmhd->bshd', p_c, mv).reshape(B, S, X_W).astype(dt)
        y_c = (o_c * jax.nn.silu(xz)) @ w_br_mem[l]

        g = jax.nn.sigmoid(gates.astype(jnp.float32)).reshape(B, S, N_BRANCH, D_MODEL)
        merged = (g[:, :, 0] * y_a.astype(jnp.float32) + g[:, :, 1] * y_b.astype(jnp.float32)
                  + g[:, :, 2] * y_c.astype(jnp.float32)).astype(dt)
        x = x + merged @ w_out[l]
    return rmsnorm(x, g_final)
```

```python
from contextlib import ExitStack
import math
import numpy as np
import concourse.bass as bass
import concourse.mybir as mybir
from concourse.bass_utils import run_bass_kernel_spmd

F32 = mybir.dt.float32
BF16 = mybir.dt.bfloat16
AF = mybir.ActivationFunctionType
ALU = mybir.AluOpType

NCORES = 8
D = 1024
S = 2048
T = 512
TPS = S // T
NG = 34
PG = 512
ENGS = ("pe", "act", "dve", "pool", "sp")
EPS = 1e-6

O_GQ, O_GK, O_GV, O_GZ, O_GLR = 0, 512, 1024, 2048, 3072
O_SQ, O_SK, O_SV, O_SZ, O_XQ, O_XZ, O_GT = 3088, 4112, 4368, 4624, 5648, 6672, 7696
G_GQ, G_GK, G_GZ, G_SQ, G_SK, G_SZ, G_XQ, G_XZ, G_GT, G_GV, G_SV = 0, 1, 2, 4, 6, 7, 9, 11, 13, 19, 21
G_BRA, G_BRB, G_BRC, G_OUT, G_MK, G_MV = 22, 24, 26, 28, 30, 32
C_ID, C_ONE, C_PERM, C_TRI, C_NTRI, C_COS, C_SIN, C_NEGC, C_END = 0, 128, 256, 384, 896, 1408, 3456, 5504, 6016
PF_BIAS, PF_GLA, PF_SINK, PF_GLR, PF_GMX, PF_GMEM, PF_GFIN, PF_END = 0, 76, 78, 94, 96, 1120, 2144, 3168


_OFFS = {}
_DBG = False
_ALLOCS = []
_SNAP = -1
_STOP = 0


class _Buf:
    __slots__ = ("w", "rs", "psum")

    def __init__(self, psum):
        self.w = None
        self.rs = []
        self.psum = psum


class _Op:
    __slots__ = ("eng", "fn", "idx", "dma", "dmaval", "deps", "signal", "sigval")

    def __init__(self, eng, fn, idx, dma):
        self.eng, self.fn, self.idx, self.dma = eng, fn, idx, dma
        self.dmaval = 0
        self.deps = []
        self.signal = False
        self.sigval = 0


class _DmaSem:
    def __init__(self, sem):
        self.sem = sem
        self.count = 0


class Prog:
    def __init__(self):
        self.ops = {e: [] for e in ENGS}
        self.bufs = {}

    def _buf(self, k):
        b = self.bufs.get(k)
        if b is None:
            b = self.bufs[k] = _Buf(isinstance(k, tuple))
        return b

    def add(self, eng, fn, r=(), w=(), dma=None):
        op = _Op(eng, fn, len(self.ops[eng]), dma)
        if dma is not None:
            dma.count += 16
            op.dmaval = dma.count
        deps = set()
        for k in r:
            b = self._buf(k)
            if b.w is not None:
                deps.add(b.w)
            if b.psum:
                for o in b.rs:
                    if o.eng != eng:
                        deps.add(o)
        for k in w:
            b = self._buf(k)
            if b.w is not None:
                deps.add(b.w)
            deps.update(b.rs)
        for d in deps:
            if d is op:
                continue
            if d.dma is not None or op.dma is not None or d.eng != eng:
                need = True
            elif eng == "pe":
                need = False
            else:
                need = (op.idx - d.idx) <= 2
            if need:
                op.deps.append(d)
                if d.dma is None:
                    d.signal = True
        for k in r:
            self._buf(k).rs.append(op)
        for k in w:
            b = self._buf(k)
            b.w = op
            b.rs = []
        self.ops[eng].append(op)
        return op

    def emit(self, nc, engsem):
        for e in ENGS:
            n = 0
            for op in self.ops[e]:
                if op.signal:
                    n += 1
                    op.sigval = n
        handles = {"pe": "tensor", "act": "scalar", "dve": "vector", "pool": "gpsimd", "sp": "sync"}
        with nc.Block() as block:
            for e in ENGS:
                ops = self.ops[e]

                def body(h, ops=ops, e=e):
                    waited = {}
                    for op in ops:
                        req = {}
                        for d in op.deps:
                            if d.dma is not None:
                                s, v = d.dma.sem, d.dmaval
                            else:
                                s, v = engsem[d.eng], d.sigval
                            key = id(s)
                            if key not in req or req[key][1] < v:
                                req[key] = (s, v)
                        for key, (s, v) in req.items():
                            if waited.get(key, 0) < v:
                                h.wait_ge(s, v)
                                waited[key] = v
                        if op.fn is None:
                            continue
                        ins = op.fn(h)
                        if op.dma is not None:
                            ins.then_inc(op.dma.sem, 16)
                        elif op.signal:
                            ins.then_inc(engsem[e], 1)

                getattr(block, handles[e])(body)


class TT:
    def __init__(self, arena, off, shape, dt, parts=128):
        self.off, self.shape, self.dt = off, tuple(shape), dt
        self.es = 2 if dt == BF16 else 4
        n = 1
        for s in shape:
            n *= s
        self.nbytes = n * self.es
        assert off % 4 == 0
        base = arena[0:parts, off // 2:(off + self.nbytes) // 2]
        if dt == F32:
            base = base.bitcast(F32)
        if len(shape) == 2:
            base = base.rearrange("p (a b) -> p a b", b=shape[1])
        elif len(shape) == 3:
            base = base.rearrange("p (a b c) -> p a b c", b=shape[1], c=shape[2])
        self.ap = base
        self.all = list(range(off // PG, (off + self.nbytes + PG - 1) // PG))

    def ch(self, i, n=1):
        cb = self.nbytes // self.shape[0]
        lo = self.off + i * cb
        hi = lo + n * cb
        return list(range(lo // PG, (hi + PG - 1) // PG))


def build_nc(NT=2 * TPS):
    nc = bass.Bass("TRN2", target_bir_lowering=False)
    x_d = nc.dram_tensor("x", [2, S, D], F32, kind="ExternalInput").ap()
    mem_d = nc.dram_tensor("mem", [2, 256, D], F32, kind="ExternalInput").ap()
    w_d = nc.dram_tensor("w_all", [D, NG * 512], F32, kind="ExternalInput").ap()
    cb_d = nc.dram_tensor("cb", [128, C_END], F32, kind="ExternalInput").ap()
    pf_d = nc.dram_tensor("pf", [128, PF_END], F32, kind="ExternalInput").ap()
    brow_d = nc.dram_tensor("brow", [1, 1536], F32, kind="ExternalInput").ap()
    wup_d = nc.dram_tensor("wup", [17, 512], F32, kind="ExternalInput").ap()
    out_d = nc.dram_tensor("out", [2, S, D], F32, kind="ExternalOutput").ap()

    es = ExitStack()
    with es:
        ARENA = 207 * 1024
        arena = es.enter_context(nc.sbuf_tensor("arena", [128, ARENA // 2], BF16))
        ps = [es.enter_context(nc.psum_tensor(f"ps{i}", [128, 512], F32)) for i in range(8)]
        engsem = {e: es.enter_context(nc.semaphore(f"s_{e}")) for e in ENGS}

        def dsem(name):
            return _DmaSem(es.enter_context(nc.semaphore(name)))

        wsem = [dsem(f"w{i}") for i in range(3)]
        csem, csem2, xsem, msem = dsem("cst"), dsem("cst2"), dsem("xld"), dsem("mld")
        osem = [dsem(f"o{i}") for i in range(4)]
        xrsem = [dsem(f"xr{i}") for i in range(4)]

        P = Prog()
        cur = [0]

        def al(shape, dt, at=None, parts=128):
            if at is None:
                nb_ = (2 if dt == BF16 else 4)
                for s_ in shape:
                    nb_ *= s_
                algn = PG if nb_ >= PG else 64
                off = (cur[0] + algn - 1) // algn * algn
                t = TT(arena, off, shape, dt, parts)
                cur[0] = off + t.nbytes
            else:
                t = TT(arena, at, shape, dt, parts)
            assert t.off + t.nbytes <= ARENA, (t.off, t.nbytes)
            _ALLOCS.append((t.off, t.nbytes, shape, at))
            return t

        cb = al([C_END], BF16)
        pf = al([PF_END], F32)
        brow = al([1536], BF16, parts=1)
        wup = al([512], BF16, parts=17)
        bhalf = al([24], F32)
        esink = al([16], F32)
        wbuf = [al([8, 512], BF16) for _ in range(3)]
        x_tm = al([4, 1024], F32)
        hT = al([8, 512], BF16)
        junk = al([1024], BF16)
        xn = al([1024], BF16)
        ss = al([12], F32)
        lnss = al([12], F32)
        rstd_x = al([12], F32)
        zact = al([8, 512], BF16)
        qbuf = al([8, 512], BF16)
        bact = al([8, 512], BF16)
        tg = al([8, 512], BF16)
        v_tm = al([4, 1024], BF16)
        krot = al([4, 512], BF16)
        khi = al([4, 512], BF16)
        kprev = al([4, 128], BF16)
        kprev_hi = al([4, 128], BF16)
        sv = al([5, 4, 66], BF16)
        _OFFS.update(krot=krot.off, kprev=kprev.off, sv=sv.off)
        glr = al([512], BF16, parts=17)
        macc = al([8, 512], F32)
        mtmp = [al([512], F32) for _ in range(2)]
        xres = [al([1024], F32, at=macc.off + i * 4096) for i in range(4)]
        outt = xres
        mkT = al([8, 256], BF16)
        mv = al([2, 1024], BF16)
        memnT = al([8, 256], BF16)
        scr0 = cur[0] = (cur[0] + PG - 1) // PG * PG
        nla = al([4, 512], BF16)
        etmp = al([512], F32)
        eb = al([4, 4, 128], BF16)
        einv = al([4, 4, 128], BF16)
        dec = al([4, 4], F32)
        qdec = al([4, 512], BF16)
        kinv = al([4, 512], BF16)
        attm = al([4, 128], BF16)
        kinv_tm = al([4, 128], BF16)
        sq = [al([4, 128], BF16) for _ in range(2)]
        lnv = al([4, 128], F32)
        rstd = al([4, 128], F32)
        tgl = [al([4, 128], BF16) for _ in range(2)]
        kdec = al([4, 128], BF16)
        S_f = al([4, 256], F32)
        S_b = al([4, 256], BF16)
        _OFFS.update(S_f=S_f.off, S_b=S_b.off)
        scr_end = cur[0]
        cur[0] = scr0
        xs = [al([512], BF16) for _ in range(2)]
        t1 = [al([512], F32) for _ in range(2)]
        ur = [al([512], F32) for _ in range(2)]
        Pt = [al([4, 128], BF16) for _ in range(4)]
        o_tm = al([1024], BF16)
        den4 = al([16], F32)
        rec4 = al([16], F32)
        assert cur[0] <= S_f.off, (cur[0], S_f.off)
        cur[0] = scr0
        mem_tm = al([2, 1024], F32)
        Px = [al([512], BF16) for _ in range(2)]
        recx = al([512], F32)
        wx = [al([512], F32) for _ in range(2)]
        assert cur[0] <= S_f.off, (cur[0], S_f.off)
        cur[0] = scr_end
        dbg = junk
        _OFFS.update(dbg=dbg.off, end=cur[0])

        cbv = cb.ap
        ident = cbv[:, C_ID:C_ID + 128]
        ones = cbv[:, C_ONE:C_ONE + 128]
        permM = cbv[:, C_PERM:C_PERM + 128]
        tri = cbv[:, C_TRI:C_TRI + 128]
        tri4 = cbv[:, C_TRI:C_TRI + 512].rearrange("p (a b) -> p a b", b=128)
        negp = cbv[:, C_NTRI:C_NTRI + 512]
        negc = cbv[:, C_NEGC:C_NEGC + 512]
        pfv = pf.ap
        gmx = pfv[:, PF_GMX:PF_GMX + 1024].rearrange("p (a b) -> p a b", b=128)
        gmemx = pfv[:, PF_GMEM:PF_GMEM + 1024].rearrange("p (a b) -> p a b", b=128)
        gfin = pfv[:, PF_GFIN:PF_GFIN + 1024]

        bank = [0]

        def nb():
            b = bank[0]
            bank[0] = (b + 1) % 8
            return b

        def PS(b):
            return ("ps", b)

        def dma(q, out, in_, w, r, sem):
            P.add(q, lambda h, out=out, in_=in_: h.dma_start(out=out, in_=in_), r=r, w=w, dma=sem)

        dma("pool", cb.ap, cb_d, cb.all, [], csem)
        dma("sp", pf.ap, pf_d, pf.all, [], csem2)
        dma("pool", brow.ap, brow_d, brow.all, [], dsem("cst3"))
        dma("pool", wup.ap, wup_d, wup.all, [], dsem("cst4"))
        P.add("dve", lambda h: h.tensor_scalar(out=bhalf.ap, in0=pfv[:, PF_BIAS + 52:PF_BIAS + 76], scalar1=0.5,
                                               scalar2=None, op0=ALU.mult), r=pf.all, w=bhalf.all)
        P.add("act", lambda h: h.activation(out=esink.ap, in_=pfv[:, PF_SINK:PF_SINK + 16], func=AF.Exp),
              r=pf.all, w=esink.all)
        P.add("pool", lambda h: h.memset(sv.ap, 1.0), w=sv.all)
        P.add("pool", lambda h: h.memset(glr.ap, 1.0), w=glr.all)
        for z_ in (krot, khi, kprev, kprev_hi):
            P.add("pool", lambda h, z_=z_: h.memset(z_.ap, 0.0), w=z_.all)

        wseq = []
        MEMG = [G_MK, G_MK + 1, G_MV, G_MV + 1]
        for t in range(NT):
            if t == 0:
                wseq += MEMG
            wseq += [G_SV, G_GQ, G_GK, G_GZ, G_GZ + 1, G_GV, G_GV + 1, G_GT, G_GT + 1, G_BRA, G_BRA + 1]
            wseq += [G_SQ, G_SQ + 1, G_SK, G_SZ, G_SZ + 1, G_GT + 2, G_GT + 3, G_BRB, G_BRB + 1]
            wseq += [G_XQ, G_XQ + 1, G_XZ, G_XZ + 1, G_GT + 4, G_GT + 5]
            wseq += [G_BRC, G_BRC + 1]
            if t + 1 < NT and (t + 1) % TPS == 0:
                wseq += MEMG
            wseq += [G_OUT, G_OUT + 1]
        wstate = {"loaded": 0, "used": 0}

        def w_load_upto(n):
            while wstate["loaded"] < min(n, len(wseq)):
                u = wstate["loaded"]
                g = wseq[u]
                slot = u % 3
                src = w_d[:, g * 512:(g + 1) * 512].rearrange("(k p) c -> p k c", p=128)
                dma("pool", wbuf[slot].ap, src, wbuf[slot].all, [], wsem[slot])
                wstate["loaded"] += 1

        def use_w(g, ahead=3):
            u = wstate["used"]
            assert wseq[u] == g, (u, wseq[u], g)
            w_load_upto(u + ahead)
            wstate["used"] += 1
            return wbuf[u % 3]

        def fm_group_gen(g, src, evac, n=512, nj=4, bank=None):
            wb = use_w(g)
            for j in range(nj):
                b = nb() if bank is None else bank

                def f(h, wb=wb, j=j, b=b):
                    for k in range(8):
                        ins = h.matmul(ps[b][:, 0:n], wb.ap[:, k, j * 128:(j + 1) * 128], src.ap[:, k, 0:n],
                                       start=(k == 0), stop=(k == 7))
                    return ins

                P.add("pe", f, r=wb.all + src.all, w=[PS(b)])
                evac(j, b)
                yield

        def fm_group(*a, **k):
            for _ in fm_group_gen(*a, **k):
                pass

        def tm_group(g, src, evac, ntb=4, ncol=512, bias_col=None):
            wb = use_w(g)
            for tb in range(ntb):
                b = nb()

                def f(h, wb=wb, tb=tb, b=b):
                    for k in range(8):
                        ins = h.matmul(ps[b][:, 0:ncol], src.ap[:, k, tb * 128:(tb + 1) * 128], wb.ap[:, k, 0:ncol],
                                       start=(k == 0), stop=(k == 7 and bias_col is None))
                    if bias_col is not None:
                        ins = h.matmul(ps[b][:, 0:ncol], cbv[0:1, C_ONE:C_ONE + 128],
                                       brow.ap[0:1, bias_col:bias_col + ncol], start=False, stop=True)
                    return ins

                P.add("pe", f, r=wb.all + src.all + cb.all + brow.all, w=[PS(b)])
                evac(tb, b)
            return wb

        def bias(c):
            return pfv[:, PF_BIAS + c:PF_BIAS + c + 1]

        def act_evac(dst_t, func, gbase, scale=1.0, bias_t=None):
            def ev(j, b, c0=0):
                c = c0 + j
                bap = bias(gbase * 4 + j) if bias_t is None else bias_t(j)
                P.add("act", lambda h, c=c, b=b, bap=bap: h.activation(out=dst_t.ap[:, c, :], in_=ps[b][:, :], func=func,
                                                                        bias=bap, scale=scale),
                      r=[PS(b)] + pf.all + bhalf.all, w=dst_t.ch(c))
            return ev

        def norm_transpose(src_t, nblk, gx, dstT, dcols, phase="ab", sc=0, prescale=False):
            nj_ = 1024 // dcols
            if "a" in phase:
                for tb in range(nblk):
                    if phase == "a":
                        jout, jw = o_tm.ap, o_tm.all
                        jin = src_t.ap[:, tb, :]
                    else:
                        jout, jw = dstT.ap[:, 0:nj_, :], dstT.ch(0, nj_)
                        jin = src_t.ap[:, tb, :].rearrange("p (a b) -> p a b", b=dcols)
                    P.add("act", lambda h, tb=tb, jout=jout, jin=jin: h.activation(
                        out=jout, in_=jin, func=AF.Square, accum_out=ss.ap[:, sc + tb:sc + tb + 1]),
                          r=src_t.ch(tb), w=jw + ss.all)
                P.add("act", lambda h: h.activation(out=lnss.ap[:, sc:sc + nblk], in_=ss.ap[:, sc:sc + nblk], func=AF.Ln,
                                                    bias=EPS, scale=1.0 / 1024.0), r=ss.all, w=lnss.all)
                P.add("act", lambda h: h.activation(out=rstd_x.ap[:, sc:sc + nblk], in_=lnss.ap[:, sc:sc + nblk],
                                                    func=AF.Exp, scale=-0.5), r=lnss.all, w=rstd_x.all)
            def scale_blk(tb):
                xb = (xn, junk)[tb % 2]
                P.add("dve", lambda h, tb=tb, xb=xb: h.tensor_scalar(out=xb.ap, in0=src_t.ap[:, tb, :],
                                                                     scalar1=rstd_x.ap[:, sc + tb:sc + tb + 1],
                                                                     scalar2=None, op0=ALU.mult),
                      r=src_t.ch(tb) + rstd_x.all, w=xb.all)
            if phase == "a":
                if prescale:
                    scale_blk(0)
                    scale_blk(1)
                return
            for tb in range(nblk):
                xb = (xn, junk)[tb % 2]
                if not (phase == "b" and prescale and tb < 2):
                    scale_blk(tb)
                for half in range(2):
                    b = nb()

                    def f(h, b=b, half=half, xb=xb):
                        for c in range(4):
                            k = half * 4 + c
                            ins = h.matmul(ps[b][:, c * 128:(c + 1) * 128], xb.ap[:, k * 128:(k + 1) * 128], ident,
                                           start=True, stop=True)
                        return ins

                    P.add("pe", f, r=xb.all + cb.all, w=[PS(b)])
                    P.add("dve", lambda h, b=b, half=half, tb=tb: h.tensor_tensor(
                        out=dstT.ap[:, half * 4:half * 4 + 4, tb * 128:(tb + 1) * 128],
                        in0=ps[b][:, :].rearrange("p (a b) -> p a b", b=128),
                        in1=gx[:, half * 4:half * 4 + 4, :], op=ALU.mult),
                          r=[PS(b)] + pf.all, w=dstT.ch(half * 4, 4))

        def gates_gen(bi, bank=None):
            for half in range(2):
                gg = G_GT + 2 * bi + half

                def ev(j, b, half=half, gg=gg):
                    c = half * 4 + j
                    P.add("act", lambda h, c=c, b=b, gg=gg, j=j: h.activation(
                        out=tg.ap[:, c, :], in_=ps[b][:, :], func=AF.Tanh,
                        bias=bhalf.ap[:, (gg - G_GT) * 4 + j:(gg - G_GT) * 4 + j + 1], scale=0.5),
                          r=[PS(b)] + bhalf.all, w=tg.ch(c))
                yield from fm_group_gen(gg, hT, ev, bank=bank)

        def branch_mm(bi, gbr):
            for half in range(2):
                def ev(j, b, half=half):
                    c = half * 4 + j
                    if bi == 0:
                        P.add("dve", lambda h, c=c, b=b: h.scalar_tensor_tensor(
                            out=macc.ap[:, c, :], in0=tg.ap[:, c, :], scalar=1.0, in1=ps[b][:, :],
                            op0=ALU.add, op1=ALU.mult), r=[PS(b)] + tg.ch(c), w=macc.ch(c))
                    else:
                        mt = mtmp[c % 2]
                        P.add("dve", lambda h, c=c, b=b, mt=mt: h.scalar_tensor_tensor(
                            out=mt.ap, in0=tg.ap[:, c, :], scalar=1.0, in1=ps[b][:, :],
                            op0=ALU.add, op1=ALU.mult), r=[PS(b)] + tg.ch(c), w=mt.all)
                        if bi == 1:
                            P.add("pool", lambda h, c=c, mt=mt: h.tensor_tensor(
                                out=macc.ap[:, c, :], in0=macc.ap[:, c, :], in1=mt.ap, op=ALU.add),
                                  r=mt.all + macc.ch(c), w=macc.ch(c))
                        else:
                            P.add("pool", lambda h, c=c, mt=mt: h.tensor_tensor(
                                out=qbuf.ap[:, c, :], in0=macc.ap[:, c, :], in1=mt.ap, op=ALU.add),
                                  r=mt.all + macc.ch(c), w=qbuf.ch(c))
                fm_group(gbr + half, bact, ev)

        def load_x(t):
            sq_i, tok0 = t // TPS, (t % TPS) * T
            dma("sp", x_tm.ap, x_d[sq_i, tok0:tok0 + T, :].rearrange("(tb p) d -> p tb d", p=128), x_tm.all, [], xsem)

        def prep_tile(t, stats_done=False):
            sq_i = t // TPS
            if t % TPS == 0:
                dma("sp", mem_tm.ap, mem_d[sq_i].rearrange("(mb p) d -> p mb d", p=128), mem_tm.all, [], msem)
                norm_transpose(mem_tm, 2, gmemx, memnT, 256, sc=8)
                for half in range(2):
                    def ev(j, b, half=half):
                        c = half * 4 + j
                        P.add("act", lambda h, c=c, b=b: h.copy(out=mkT.ap[:, c, :], in_=ps[b][:, 0:256]),
                              r=[PS(b)], w=mkT.ch(c))
                    fm_group(G_MK + half, memnT, ev, n=256)
                for half in range(2):
                    def ev(mb, b, half=half):
                        P.add("dve", lambda h, mb=mb, b=b: h.tensor_copy(out=mv.ap[:, mb, half * 512:(half + 1) * 512],
                                                                         in_=ps[b][:, :]),
                              r=[PS(b)], w=mv.ch(mb))
                    tm_group(G_MV + half, memnT, ev, ntb=2)
                P.add("pool", lambda h: h.memset(S_f.ap, 0.0), w=S_f.all)
                P.add("pool", lambda h: h.memset(S_b.ap, 0.0), w=S_b.all)
            norm_transpose(x_tm, 4, gmx, hT, 512, phase="b" if stats_done else "ab", prescale=(t % TPS != 0))
            if t + 1 < NT:
                load_x(t + 1)

        for t in range(NT):
            sq_i = t // TPS
            ti = t % TPS
            tok0 = ti * T

            if t == 0:
                load_x(0)
                prep_tile(0)
            rope_pend = []

            def rope_flush():
                while rope_pend:
                    rope_pend.pop(0)()

            def rope_evac(dst, gbase, pbank=None):
                def ev(j, b, c0=0):
                    c = c0 + j
                    x_ = xs[c % 2]
                    t_ = t1[c % 2]
                    u_ = ur[c % 2]
                    cosap = cbv[:, C_COS + tok0:C_COS + tok0 + T]
                    sinap = cbv[:, C_SIN + tok0:C_SIN + tok0 + T]
                    P.add("act", lambda h, b=b, x_=x_, j=j: h.activation(out=x_.ap, in_=ps[b][:, :], func=AF.Identity,
                                                                        bias=bias(gbase * 4 + j), scale=1.0),
                          r=[PS(b)] + pf.all, w=x_.all)
                    P.add("dve", lambda h, b=b, t_=t_, cosap=cosap, j=j: h.scalar_tensor_tensor(
                        out=t_.ap, in0=ps[b][:, :], scalar=bias(gbase * 4 + j), in1=cosap, op0=ALU.add, op1=ALU.mult),
                          r=[PS(b)] + pf.all + cb.all, w=t_.all)

                    def tail(c=c, x_=x_, t_=t_, u_=u_, sinap=sinap):
                        b2 = nb() if pbank is None else pbank
                        P.add("pe", lambda h, b2=b2, x_=x_: h.matmul(ps[b2][:, :], permM, x_.ap, start=True, stop=True),
                              r=x_.all + cb.all, w=[PS(b2)])
                        P.add("dve", lambda h, b2=b2, u_=u_, sinap=sinap: h.tensor_tensor(
                            out=u_.ap, in0=ps[b2][:, :], in1=sinap, op=ALU.mult),
                              r=[PS(b2)] + cb.all, w=u_.all)
                        if dst is krot:
                            P.add("pool", lambda h, c=c, t_=t_, u_=u_: h.tensor_tensor(
                                out=krot.ap[0:64, c, :], in0=t_.ap[0:64, :], in1=u_.ap[0:64, :], op=ALU.add),
                                  r=t_.all + u_.all, w=krot.ch(c))
                            P.add("pool", lambda h, c=c, t_=t_, u_=u_: h.tensor_tensor(
                                out=khi.ap[64:128, c, :], in0=t_.ap[64:128, :], in1=u_.ap[64:128, :], op=ALU.add),
                                  r=t_.all + u_.all, w=khi.ch(c))
                        else:
                            P.add("pool", lambda h, c=c, t_=t_, u_=u_: h.tensor_tensor(out=dst.ap[:, c, :], in0=t_.ap,
                                                                                      in1=u_.ap, op=ALU.add),
                                  r=t_.all + u_.all, w=dst.ch(c))
                    prev = list(rope_pend)
                    del rope_pend[:]
                    rope_pend.append(tail)
                    for f_ in prev:
                        f_()
                return ev

            if _SNAP == 0 and t == 1:
                P.add('pool', lambda h: h.tensor_copy(out=dbg.ap[:, 0:512], in_=kprev.ap.rearrange('p a b -> p (a b)')), r=kprev.all, w=dbg.all)
            if _STOP == 2:
                break
            qraw, kraw = qbuf, qbuf

            def ev_sv(tb, b):
                P.add("dve", lambda h, tb=tb, b=b: h.tensor_copy(
                    out=sv.ap[:, tb + 1, :, 0:64], in_=ps[b][:, 0:256].rearrange("p (g d) -> p g d", d=64)),
                      r=[PS(b)], w=sv.ch(tb + 1))
            wb = tm_group(G_SV, hT, ev_sv, ncol=256, bias_col=1024)
            b = nb()

            def f(h, wb=wb, b=b):
                for k in range(8):
                    ins = h.matmul(ps[b][0:16, :], wb.ap[:, k, 256:272], hT.ap[:, k, :], start=(k == 0), stop=(k == 7))
                return ins
            P.add("pe", f, r=wb.all + hT.all, w=[PS(b)])
            P.add("act", lambda h, b=b: h.activation(out=glr.ap[0:16, :], in_=ps[b][0:16, :], func=AF.Identity,
                                                     bias=pfv[0:16, PF_GLR:PF_GLR + 1], scale=1.0),
                  r=[PS(b)] + pf.all, w=glr.all)

            def s1_z(tb):
                cs = slice(tb * 128, (tb + 1) * 128)
                b = nb()
                P.add("pe", lambda h, b=b, cs=cs: h.matmul(ps[b][:, :], glr.ap[0:17, cs], wup.ap[0:17, :],
                                                           start=True, stop=True),
                      r=glr.all + wup.all, w=[PS(b)])
                P.add("act", lambda h, b=b: h.activation(out=etmp.ap, in_=ps[b][:, :], func=AF.Exp, scale=-1.0),
                      r=[PS(b)], w=etmp.all)
                P.add("act", lambda h, tb=tb: h.activation(out=nla.ap[:, tb, :], in_=etmp.ap, func=AF.Ln, bias=1.0,
                                                           scale=1.0),
                      r=etmp.all, w=nla.ch(tb))

            def s1_c(tb):
                b = nb()

                def f(h, b=b, tb=tb):
                    for hh in range(4):
                        ins = h.matmul(ps[b][:, hh * 128:(hh + 1) * 128], nla.ap[:, tb, hh * 128:(hh + 1) * 128], tri,
                                       start=True, stop=True)
                    return ins
                P.add("pe", f, r=nla.ch(tb) + cb.all, w=[PS(b)])
                pb = ps[b][:, :].rearrange("p (a b) -> p a b", b=128)
                P.add("act", lambda h, pb=pb, tb=tb: h.activation(out=eb.ap[:, tb, :, :], in_=pb, func=AF.Exp,
                                                                  scale=-1.0 / 16.0, bias=-0.5 * math.log(128.0)),
                      r=[PS(b)], w=eb.ch(tb))
                P.add("act", lambda h, pb=pb, tb=tb: h.activation(out=einv.ap[:, tb, :, :], in_=pb, func=AF.Exp,
                                                                  scale=1.0 / 16.0),
                      r=[PS(b)], w=einv.ch(tb))
                P.add("act", lambda h, pb=pb, tb=tb: h.activation(out=dec.ap[:, tb, :], in_=pb[:, :, 127], func=AF.Exp,
                                                                  scale=-1.0 / 16.0),
                      r=[PS(b)], w=dec.all)

            fm_group(G_GQ, hT, lambda j, b: act_evac(qbuf, AF.Identity, G_GQ)(j, b, 0))
            for tb in range(4):
                s1_z(tb)
            fm_group(G_GK, hT, lambda j, b: act_evac(qbuf, AF.Identity, G_GK)(j, b, 4))
            s1_c(0)
            s1_c(1)
            fm_group(G_GZ, hT, lambda j, b: act_evac(zact, AF.Silu, G_GZ)(j, b, 0))
            s1_c(2)
            s1_c(3)
            fm_group(G_GZ + 1, hT, lambda j, b: act_evac(zact, AF.Silu, G_GZ + 1)(j, b, 4))
            for half in range(2):
                def ev(tb, b, half=half):
                    P.add("dve", lambda h, tb=tb, b=b: h.tensor_copy(out=v_tm.ap[:, tb, half * 512:(half + 1) * 512],
                                                                     in_=ps[b][:, :]),
                          r=[PS(b)], w=v_tm.ch(tb))
                tm_group(G_GV + half, hT, ev, bias_col=half * 512)

            if _STOP == 3:
                break
            gg0 = gates_gen(0, bank=7)
            for tb in range(4):
                cs = slice(tb * 128, (tb + 1) * 128)
                P.add("dve", lambda h, cs=cs, tb=tb: h.tensor_tensor(out=qdec.ap[:, :, cs], in0=qbuf.ap[:, 0:4, cs],
                                                                     in1=eb.ap[:, tb, :, :], op=ALU.mult),
                      r=qbuf.ch(0, 4) + eb.ch(tb), w=qdec.all)
                P.add("pool", lambda h, cs=cs, tb=tb: h.tensor_tensor(out=kinv.ap[:, :, cs], in0=qbuf.ap[:, 4:8, cs],
                                                                      in1=einv.ap[:, tb, :, :], op=ALU.mult),
                      r=qbuf.ch(4, 4) + einv.ch(tb), w=kinv.all)

            B_ATT, B_KT, B_O, B_SS, B_S = 0, 1, (2, 3), 4, (5, 6)

            def gla_norm_tail(tb, cs):
                def f(h):
                    for hh in range(4):
                        o = ps[B_SS][:, hh * 128:(hh + 1) * 128]
                        h.matmul(o, ones, sq[0].ap[:, hh, :], start=True, stop=False)
                        ins = h.matmul(o, ones, sq[1].ap[:, hh, :], start=False, stop=True)
                    return ins
                P.add("pe", f, r=sq[0].all + sq[1].all + cb.all, w=[PS(B_SS)])
                P.add("act", lambda h: h.activation(out=lnv.ap,
                                                    in_=ps[B_SS][:, :].rearrange("p (a b) -> p a b", b=128),
                                                    func=AF.Ln, bias=EPS, scale=1.0 / 256.0),
                      r=[PS(B_SS)], w=lnv.all)
                P.add("act", lambda h: h.activation(out=rstd.ap, in_=lnv.ap, func=AF.Exp, scale=-0.5),
                      r=lnv.all, w=rstd.all)
                for vc in range(2):
                    P.add("dve", lambda h, vc=vc: h.scalar_tensor_tensor(
                        out=tgl[vc].ap, in0=ps[B_O[vc]][:, :].rearrange("p (a b) -> p a b", b=128),
                        scalar=pfv[:, PF_GLA + vc:PF_GLA + vc + 1], in1=rstd.ap, op0=ALU.mult, op1=ALU.mult),
                          r=[PS(B_O[vc])] + pf.all + rstd.all, w=tgl[vc].all)
                    P.add("pool", lambda h, vc=vc, cs=cs: h.tensor_tensor(
                        out=bact.ap[:, vc::2, cs], in0=tgl[vc].ap, in1=zact.ap[:, vc::2, cs], op=ALU.mult),
                          r=tgl[vc].all + zact.all, w=bact.all)

            def g_att(tb):
                cs = slice(tb * 128, (tb + 1) * 128)

                def f(h, cs=cs):
                    for hh in range(4):
                        ins = h.matmul(ps[B_ATT][:, hh * 128:(hh + 1) * 128], kinv.ap[:, hh, cs], qdec.ap[:, hh, cs],
                                       start=True, stop=True)
                    return ins
                P.add("pe", f, r=kinv.all + qdec.all, w=[PS(B_ATT)])
                P.add("dve", lambda h: h.tensor_tensor(out=attm.ap,
                                                       in0=ps[B_ATT][:, :].rearrange("p (a b) -> p a b", b=128),
                                                       in1=tri4, op=ALU.mult),
                      r=[PS(B_ATT)] + cb.all, w=attm.all)
                P.add("dve", lambda h, cs=cs, tb=tb: h.tensor_tensor(
                    out=kdec.ap, in0=kinv.ap[:, :, cs], in1=dec.ap[:, tb, :].unsqueeze(2).to_broadcast([128, 4, 128]),
                    op=ALU.mult), r=kinv.all + dec.all, w=kdec.all)

            def g_kT(tb):
                def f(h):
                    for hh in range(4):
                        ins = h.matmul(ps[B_KT][:, hh * 128:(hh + 1) * 128], kdec.ap[:, hh, :], ident,
                                       start=True, stop=True)
                    return ins
                P.add("pe", f, r=kdec.all + cb.all, w=[PS(B_KT)])
                P.add("act", lambda h: h.copy(out=kinv_tm.ap,
                                              in_=ps[B_KT][:, :].rearrange("p (a b) -> p a b", b=128)),
                      r=[PS(B_KT)], w=kinv_tm.all)

            def g_smm(tb):
                for hp in range(2):
                    def f(h, hp=hp, tb=tb):
                        for q in range(2):
                            hh = hp * 2 + q
                            ins = h.matmul(ps[B_S[hp]][:, q * 256:(q + 1) * 256], kinv_tm.ap[:, hh, :],
                                           v_tm.ap[:, tb, hh * 256:(hh + 1) * 256], start=True, stop=True)
                        return ins
                    P.add("pe", f, r=kinv_tm.all + v_tm.ch(tb), w=[PS(B_S[hp])])

            def g_o(tb):
                cs = slice(tb * 128, (tb + 1) * 128)
                for vc in range(2):
                    def f(h, vc=vc, tb=tb, cs=cs):
                        for hh in range(4):
                            o = ps[B_O[vc]][:, hh * 128:(hh + 1) * 128]
                            vcol = hh * 256 + vc * 128
                            h.matmul(o, v_tm.ap[:, tb, vcol:vcol + 128], attm.ap[:, hh, :], start=True, stop=False)
                            ins = h.matmul(o, S_b.ap[:, hh, vc * 128:(vc + 1) * 128], qdec.ap[:, hh, cs],
                                           start=False, stop=True)
                        return ins
                    P.add("pe", f, r=v_tm.ch(tb) + attm.all + S_b.all + qdec.all, w=[PS(B_O[vc])])
                    P.add("act", lambda h, vc=vc: h.activation(
                        out=sq[vc].ap, in_=ps[B_O[vc]][:, :].rearrange("p (a b) -> p a b", b=128), func=AF.Square),
                          r=[PS(B_O[vc])], w=sq[vc].all)

            def g_supd(tb):
                for hh in range(4):
                    hp, q = hh // 2, hh % 2
                    P.add("dve", lambda h, hh=hh, hp=hp, q=q, tb=tb: h.scalar_tensor_tensor(
                        out=S_f.ap[:, hh, :], in0=S_f.ap[:, hh, :], scalar=dec.ap[:, tb, hh:hh + 1],
                        in1=ps[B_S[hp]][:, q * 256:(q + 1) * 256], op0=ALU.mult, op1=ALU.add),
                          r=[PS(B_S[hp])] + S_f.ch(hh) + dec.all, w=S_f.ch(hh))
                    P.add("pool", lambda h, hh=hh: h.tensor_copy(out=S_b.ap[:, hh, :], in_=S_f.ap[:, hh, :]),
                          r=S_f.ch(hh), w=S_b.ch(hh))

            g_att(0)
            g_kT(0)
            g_smm(0)
            for tb in range(4):
                cs = slice(tb * 128, (tb + 1) * 128)
                g_o(tb)
                if tb + 1 < 4:
                    g_att(tb + 1)
                g_supd(tb)
                next(gg0, None)
                if tb + 1 < 4:
                    g_kT(tb + 1)
                gla_norm_tail(tb, cs)
                if tb + 1 < 4:
                    g_smm(tb + 1)
                next(gg0, None)

            if _SNAP == 1 and t == 1:
                P.add('pool', lambda h: h.tensor_copy(out=dbg.ap[:, 0:512], in_=kprev.ap.rearrange('p a b -> p (a b)')), r=kprev.all, w=dbg.all)
            if _STOP == 4:
                break
            for _ in gg0:
                pass
            branch_mm(0, G_BRA)
            if _SNAP == 2 and t == 1:
                P.add('pool', lambda h: h.tensor_copy(out=dbg.ap[:, 0:512], in_=kprev.ap.rearrange('p a b -> p (a b)')), r=kprev.all, w=dbg.all)

            if _STOP == 5:
                break
            fm_group(G_SQ, hT, lambda j, b: rope_evac(qbuf, G_SQ)(j, b, 0))
            fm_group(G_SQ + 1, hT, lambda j, b: rope_evac(qbuf, G_SQ + 1)(j, b, 4))
            fm_group(G_SK, hT, lambda j, b: rope_evac(krot, G_SK)(j, b, 0))
            rope_flush()
            if _SNAP == 3 and t == 1:
                P.add('pool', lambda h: h.tensor_copy(out=dbg.ap[:, 0:512], in_=kprev.ap.rearrange('p a b -> p (a b)')), r=kprev.all, w=dbg.all)

            if _STOP == 6:
                break
            gg1 = gates_gen(1, bank=7)
            SB_SC, SB_PV, SB_TR = ((0, 1), (2, 3)), (4, 5), 6

            def swa_A(tb, g):
                cs = slice(tb * 128, (tb + 1) * 128)
                has_prev = not (ti == 0 and tb == 0)
                kbs = (["prev"] if has_prev else []) + ["cur"]
                pts = {}
                for kb in kbs:
                    b = SB_SC[g % 2][0 if kb == "prev" else 1]
                    pt = Pt[(g % 2) * 2 + (0 if kb == "prev" else 1)]
                    pts[kb] = pt
                    if kb == "cur":
                        ksrc = (lambda p0, g=g, cs=cs: (krot if p0 == 0 else khi).ap[:, g, cs])
                        kr = krot.ch(g) + khi.ch(g)
                    elif tb > 0:
                        pc = slice((tb - 1) * 128, tb * 128)
                        ksrc = (lambda p0, g=g, pc=pc: (krot if p0 == 0 else khi).ap[:, g, pc])
                        kr = krot.ch(g) + khi.ch(g)
                    else:
                        ksrc = (lambda p0, g=g: (kprev if p0 == 0 else kprev_hi).ap[:, g, :])
                        kr = kprev.all + kprev_hi.all

                    negm = negc if kb == "cur" else negp

                    def f(h, b=b, g=g, ksrc=ksrc, cs=cs, negm=negm):
                        h.matmul(ps[b][:, :], ident, negm, start=True, stop=False)
                        for j in range(4):
                            qh = 4 * g + j
                            p0 = (qh % 2) * 64
                            ins = h.matmul(ps[b][:, j * 128:(j + 1) * 128], ksrc(p0),
                                           qbuf.ap[:, qh // 2, cs], start=False, stop=(j == 3))
                        return ins
                    P.add("pe", f, r=kr + qbuf.ch(2 * g, 2) + cb.all, w=[PS(b)])
                    P.add("act", lambda h, b=b, pt=pt: h.activation(
                        out=pt.ap, in_=ps[b][:, :].rearrange("p (a b) -> p a b", b=128), func=AF.Exp, scale=0.125),
                          r=[PS(b)], w=pt.all)
                return kbs, pts

            def swa_B(tb, g, kbs, pts):
                bo_ = SB_PV[g % 2]

                def f(h, bo_=bo_, g=g, kbs=kbs, pts=pts, tb=tb):
                    for j in range(4):
                        for n_, kb in enumerate(kbs):
                            slot = tb + 1 if kb == "cur" else tb
                            ins = h.matmul(ps[bo_][:, j * 128:j * 128 + 65], pts[kb].ap[:, j, :],
                                           sv.ap[:, slot, g, 0:65], start=(n_ == 0), stop=(n_ == len(kbs) - 1))
                    return ins
                P.add("pe", f, r=sum([pts[kb].all for kb in kbs], []) + sv.ch(tb, 2), w=[PS(bo_)])
                po = ps[bo_][:, :].rearrange("p (a b) -> p a b", b=128)
                P.add("dve", lambda h, po=po, g=g: h.tensor_tensor(out=den4.ap[:, 4 * g:4 * g + 4], in0=po[:, :, 64],
                                                                   in1=esink.ap[:, 4 * g:4 * g + 4], op=ALU.add),
                      r=[PS(bo_)] + esink.all, w=den4.all)
                P.add("dve", lambda h, g=g: h.reciprocal(out=rec4.ap[:, 4 * g:4 * g + 4],
                                                         in_=den4.ap[:, 4 * g:4 * g + 4]),
                      r=den4.all, w=rec4.all)
                P.add("dve", lambda h, po=po, g=g: h.tensor_tensor(
                    out=o_tm.ap.rearrange("p (h d) -> p h d", d=64)[:, 4 * g:4 * g + 4, :], in0=po[:, :, 0:64],
                    in1=rec4.ap[:, 4 * g:4 * g + 4].unsqueeze(2).to_broadcast([128, 4, 64]), op=ALU.mult),
                      r=[PS(bo_)] + rec4.all, w=o_tm.all)

            def swa_C(tb):
                cs = slice(tb * 128, (tb + 1) * 128)
                for half in range(2):
                    b = SB_TR

                    def f(h, b=b, half=half):
                        for c in range(4):
                            k = half * 4 + c
                            ins = h.matmul(ps[b][:, c * 128:(c + 1) * 128], o_tm.ap[:, k * 128:(k + 1) * 128], ident,
                                           start=True, stop=True)
                        return ins
                    P.add("pe", f, r=o_tm.all + cb.all, w=[PS(b)])
                    P.add("dve", lambda h, b=b, half=half, cs=cs: h.tensor_tensor(
                        out=bact.ap[:, half * 4:half * 4 + 4, cs], in0=ps[b][:, :].rearrange("p (a b) -> p a b", b=128),
                        in1=zact.ap[:, half * 4:half * 4 + 4, cs], op=ALU.mult),
                          r=[PS(b)] + zact.ch(half * 4, 4), w=bact.ch(half * 4, 4))

            a0_ = swa_A(0, 0)
            fm_group(G_SZ, hT, lambda j, b: act_evac(zact, AF.Silu, G_SZ)(j, b, 0))
            fm_group(G_SZ + 1, hT, lambda j, b: act_evac(zact, AF.Silu, G_SZ + 1)(j, b, 4))
            prev_u = None
            nu = 0
            for tb in range(4):
                for g in range(4):
                    a_ = a0_ if (tb, g) == (0, 0) else swa_A(tb, g)
                    if prev_u is not None:
                        swa_B(*prev_u)
                        if prev_u[1] == 3:
                            swa_C(prev_u[0])
                    prev_u = (tb, g) + a_
                    nu += 1
                    if nu % 2 == 0:
                        next(gg1, None)
            swa_B(*prev_u)
            swa_C(3)
            P.add("pool", lambda h: h.tensor_copy(out=kprev.ap[0:64], in_=krot.ap[0:64, :, 384:512]),
                  r=krot.all, w=kprev.all)
            P.add("pool", lambda h: h.tensor_copy(out=kprev_hi.ap[64:128], in_=khi.ap[64:128, :, 384:512]),
                  r=khi.all, w=kprev_hi.all)
            P.add("pool", lambda h: h.tensor_copy(out=sv.ap[:, 0, :, 0:64], in_=sv.ap[:, 4, :, 0:64]),
                  r=sv.ch(4), w=sv.ch(0))

            for _ in gg1:
                pass
            branch_mm(1, G_BRB)

            if _STOP == 7:
                break
            if t + 1 < NT:
                norm_transpose(x_tm, 4, gmx, hT, 512, phase="a", prescale=((t + 1) % TPS != 0))
            fm_group(G_XQ, hT, lambda j, b: act_evac(qbuf, AF.Identity, G_XQ)(j, b, 0))
            fm_group(G_XQ + 1, hT, lambda j, b: act_evac(qbuf, AF.Identity, G_XQ + 1)(j, b, 4))
            fm_group(G_XZ, hT, lambda j, b: act_evac(zact, AF.Silu, G_XZ)(j, b, 0))
            fm_group(G_XZ + 1, hT, lambda j, b: act_evac(zact, AF.Silu, G_XZ + 1)(j, b, 4))
            gg2 = gates_gen(2)
            for xh in range(4):
                next(gg2, None)
                next(gg2, None)
                for mb in range(2):
                    b = nb()

                    def f(h, b=b, mb=mb, xh=xh):
                        for dc in range(2):
                            ins = h.matmul(ps[b][:, :], mkT.ap[:, xh * 2 + dc, mb * 128:(mb + 1) * 128],
                                           qbuf.ap[:, xh * 2 + dc, :], start=(dc == 0), stop=(dc == 1))
                        return ins
                    P.add("pe", f, r=mkT.ch(xh * 2, 2) + qbuf.ch(xh * 2, 2), w=[PS(b)])
                    P.add("act", lambda h, b=b, mb=mb: h.activation(out=Px[mb].ap, in_=ps[b][:, :], func=AF.Exp,
                                                                    scale=1.0 / 16.0),
                          r=[PS(b)], w=Px[mb].all)
                bd = nb()

                def f(h, bd=bd):
                    for mb in range(2):
                        ins = h.matmul(ps[bd][:, :], ones, Px[mb].ap, start=(mb == 0), stop=(mb == 1))
                    return ins
                P.add("pe", f, r=Px[0].all + Px[1].all + cb.all, w=[PS(bd)])
                P.add("act", lambda h, bd=bd: h.activation(out=recx.ap, in_=ps[bd][:, :], func=AF.Ln),
                      r=[PS(bd)], w=recx.all)
                P.add("act", lambda h: h.activation(out=recx.ap, in_=recx.ap, func=AF.Exp, scale=-1.0),
                      r=recx.all, w=recx.all)
                for dc in range(2):
                    c = xh * 2 + dc
                    b = nb()

                    def f(h, b=b, c=c):
                        for mb in range(2):
                            ins = h.matmul(ps[b][:, :], mv.ap[:, mb, c * 128:(c + 1) * 128], Px[mb].ap,
                                           start=(mb == 0), stop=(mb == 1))
                        return ins
                    P.add("pe", f, r=mv.all + Px[0].all + Px[1].all, w=[PS(b)])
                    P.add("pool", lambda h, c=c, dc=dc: h.tensor_tensor(out=wx[dc].ap, in0=recx.ap, in1=zact.ap[:, c, :],
                                                                        op=ALU.mult),
                          r=recx.all + zact.ch(c), w=wx[dc].all)
                    P.add("dve", lambda h, b=b, c=c, dc=dc: h.tensor_tensor(out=bact.ap[:, c, :], in0=ps[b][:, :],
                                                                            in1=wx[dc].ap, op=ALU.mult),
                          r=[PS(b)] + wx[dc].all, w=bact.ch(c))

            for _ in gg2:
                pass
            branch_mm(2, G_BRC)
            if t + 1 < NT:
                prep_tile(t + 1, stats_done=True)

            if _STOP == 8:
                break
            for tb in range(4):
                dma("sp", xres[tb].ap, x_d[sq_i, tok0 + tb * 128:tok0 + (tb + 1) * 128, :], xres[tb].all, [], xrsem[tb])
            for half in range(2):
                def ev(tb, b, half=half):
                    xr = xres[tb]
                    P.add("dve", lambda h, b=b, xr=xr: h.scalar_tensor_tensor(
                        out=xr.ap[:, half * 512:(half + 1) * 512], in0=ps[b][:, :], scalar=0.5,
                        in1=xr.ap[:, half * 512:(half + 1) * 512], op0=ALU.mult, op1=ALU.add),
                          r=[PS(b)] + xr.all, w=xr.all)
                tm_group(G_OUT + half, qbuf, ev)
            for tb in range(4):
                xr = xres[tb]
                P.add("act", lambda h, tb=tb, xr=xr: h.activation(out=junk.ap, in_=xr.ap, func=AF.Square,
                                                                  accum_out=ss.ap[:, 4 + tb:5 + tb]),
                      r=xr.all, w=junk.all + ss.all)
                P.add("act", lambda h, tb=tb: h.activation(out=lnss.ap[:, 4 + tb:5 + tb], in_=ss.ap[:, 4 + tb:5 + tb],
                                                           func=AF.Ln, bias=EPS, scale=1.0 / 1024.0),
                      r=ss.all, w=lnss.all)
                P.add("act", lambda h, tb=tb: h.activation(out=rstd_x.ap[:, 4 + tb:5 + tb],
                                                           in_=lnss.ap[:, 4 + tb:5 + tb], func=AF.Exp, scale=-0.5),
                      r=lnss.all, w=rstd_x.all)
                P.add("dve", lambda h, tb=tb, xr=xr: h.scalar_tensor_tensor(
                    out=xr.ap, in0=xr.ap, scalar=rstd_x.ap[:, 4 + tb:5 + tb], in1=gfin,
                    op0=ALU.mult, op1=ALU.mult), r=xr.all + rstd_x.all + pf.all, w=xr.all)
                dma("sp", out_d[sq_i, tok0 + tb * 128:tok0 + (tb + 1) * 128, :], xr.ap, [], xr.all, osem[tb])

        P.add("sp", None, w=xres[0].all + xres[1].all + xres[2].all + xres[3].all)
        assert _STOP or wstate["used"] == len(wseq)
        P.add("pe", None, r=wbuf[0].all + wbuf[1].all + wbuf[2].all)
        P.add("sp", None, r=x_tm.all + mem_tm.all + pf.all)
        P.emit(nc, engsem)
    return nc


def _host_tables():
    cbt = np.zeros((128, C_END), np.float32)
    p = np.arange(128)
    cbt[:, C_ID:C_ID + 128] = np.eye(128, dtype=np.float32)
    cbt[:, C_ONE:C_ONE + 128] = 1.0
    perm = np.where((p % 64) < 32, p + 32, p - 32)
    pm = np.zeros((128, 128), np.float32)
    pm[perm, p] = 1.0
    cbt[:, C_PERM:C_PERM + 128] = pm
    tri = (p[:, None] <= p[None, :]).astype(np.float32)
    cbt[:, C_TRI:C_TRI + 512] = np.tile(tri, (1, 4))
    cbt[:, C_NTRI:C_NTRI + 512] = np.tile(-30000.0 * tri, (1, 4))
    cbt[:, C_NEGC:C_NEGC + 512] = np.tile(-30000.0 * (1.0 - tri), (1, 4))
    inv = (10000.0 ** (-np.arange(32, dtype=np.float32) / 32)).astype(np.float32)
    ang = np.arange(S, dtype=np.float32)[None, :] * inv[p % 32][:, None]
    cbt[:, C_COS:C_COS + S] = np.cos(ang)
    sgn = np.where((p % 64) < 32, -1.0, 1.0).astype(np.float32)
    cbt[:, C_SIN:C_SIN + S] = np.sin(ang) * sgn[:, None]
    return cbt


def _col_order():
    r = np.arange
    sk = np.concatenate([np.concatenate([r(O_SK + g * 64, O_SK + (g + 1) * 64)] * 2) for g in range(4)])
    cols = [r(O_GQ, O_GQ + 512), r(O_GK, O_GK + 512), r(O_GZ, O_GZ + 1024), r(O_SQ, O_SQ + 1024), sk,
            r(O_SZ, O_SZ + 1024), r(O_XQ, O_XQ + 1024), r(O_XZ, O_XZ + 1024), r(O_GT, O_GT + 3072),
            r(O_GV, O_GV + 1024), r(O_SV, O_SV + 256), r(O_GLR, O_GLR + 16)]
    return np.concatenate(cols)


def kernel(x, mem, g_mix, g_mem, w_in, b_in, w_gla_gate_up, b_gla_gate, g_gla_norm, sinks,
           w_mem_kv, w_br_gla, w_br_swa, w_br_mem, w_out, g_final, _NT=2 * TPS, _cores=NCORES):
    f32 = np.float32
    x = np.asarray(x, f32)
    mem = np.asarray(mem, f32)
    co = _col_order()
    w_in0 = np.asarray(w_in, f32)[0]
    b_in0 = np.asarray(b_in, f32)[0]
    w_all = np.zeros((D, NG * 512), f32)
    w_all[:, :co.size] = w_in0[:, co]
    w_all[:, G_BRA * 512:G_BRA * 512 + 1024] = np.asarray(w_br_gla, f32)[0]
    w_all[:, G_BRB * 512:G_BRB * 512 + 1024] = np.asarray(w_br_swa, f32)[0]
    w_all[:, G_BRC * 512:G_BRC * 512 + 1024] = np.asarray(w_br_mem, f32)[0]
    w_all[:, G_OUT * 512:G_OUT * 512 + 1024] = np.asarray(w_out, f32)[0]
    w_all[:, G_MK * 512:G_MK * 512 + 2048] = np.asarray(w_mem_kv, f32)[0]
    b_all = np.zeros(22 * 512, f32)
    b_all[:co.size] = b_in0[co]
    pf = np.zeros((128, PF_END), f32)
    pf[:, PF_BIAS:PF_BIAS + 76] = b_all[:76 * 128].reshape(76, 128).T
    pf[:, PF_GLA:PF_GLA + 2] = np.asarray(g_gla_norm, f32)[0].reshape(2, 128).T
    pf[:, PF_SINK:PF_SINK + 16] = np.broadcast_to(np.asarray(sinks, f32)[0][None, :], (128, 16))
    pf[0:16, PF_GLR] = b_all[21 * 512 + 256:21 * 512 + 272]
    pf[:, PF_GMX:PF_GMX + 1024] = np.repeat(np.asarray(g_mix, f32)[0].reshape(8, 128).T[:, :, None], 128, 2).reshape(128, 1024)
    pf[:, PF_GMEM:PF_GMEM + 1024] = np.repeat(np.asarray(g_mem, f32)[0].reshape(8, 128).T[:, :, None], 128, 2).reshape(128, 1024)
    pf[:, PF_GFIN:PF_GFIN + 1024] = np.broadcast_to(np.asarray(g_final, f32)[None, :], (128, 1024))
    brow = np.ascontiguousarray(b_all[19 * 512:22 * 512][None, :])
    wup = np.concatenate([np.asarray(w_gla_gate_up, f32)[0], np.asarray(b_gla_gate, f32)[0][None, :]], 0)
    cbt = _host_tables()

    nc = build_nc(_NT)
    in_maps = []
    for c in range(_cores):
        in_maps.append({"x": np.ascontiguousarray(x[2 * c:2 * c + 2]), "mem": np.ascontiguousarray(mem[2 * c:2 * c + 2]),
                        "w_all": w_all, "cb": cbt, "pf": pf, "brow": brow, "wup": np.ascontiguousarray(wup)})
    res = run_bass_kernel_spmd(nc, in_maps, core_ids=list(range(_cores)))
    return np.concatenate([res.results[c]["out"] for c in range(_cores)], axis=0).astype(np.float32)
```

```python
from contextlib import ExitStack
import math
import numpy as np
import concourse.bass as bass
import concourse.mybir as mybir
from concourse.bass_utils import run_bass_kernel_spmd

F32 = mybir.dt.float32
BF16 = mybir.dt.bfloat16
AF = mybir.ActivationFunctionType
ALU = mybir.AluOpType

NCORES = 8
D = 1024
S = 2048
T = 512
TPS = S // T
NG = 34
PG = 512
ENGS = ("pe", "act", "dve", "pool", "sp")
EPS = 1e-6

O_GQ, O_GK, O_GV, O_GZ, O_GLR = 0, 512, 1024, 2048, 3072
O_SQ, O_SK, O_SV, O_SZ, O_XQ, O_XZ, O_GT = 3088, 4112, 4368, 4624, 5648, 6672, 7696
G_GQ, G_GK, G_GZ, G_SQ, G_SK, G_SZ, G_XQ, G_XZ, G_GT, G_GV, G_SV = 0, 1, 2, 4, 6, 7, 9, 11, 13, 19, 21
G_BRA, G_BRB, G_BRC, G_OUT, G_MK, G_MV = 22, 24, 26, 28, 30, 32
C_ID, C_ONE, C_PERM, C_TRI, C_NTRI, C_COS, C_SIN, C_NEGC, C_END = 0, 128, 256, 384, 896, 1408, 3456, 5504, 6016
PF_BIAS, PF_GLA, PF_SINK, PF_GLR, PF_GMX, PF_GMEM, PF_GFIN, PF_END = 0, 76, 78, 94, 96, 1120, 2144, 3168


_OFFS = {}
_DBG = False
_ALLOCS = []
_SNAP = -1
_STOP = 0


class _Buf:
    __slots__ = ("w", "rs", "psum")

    def __init__(self, psum):
        self.w = None
        self.rs = []
        self.psum = psum


class _Op:
    __slots__ = ("eng", "fn", "idx", "dma", "dmaval", "deps", "signal", "sigval")

    def __init__(self, eng, fn, idx, dma):
        self.eng, self.fn, self.idx, self.dma = eng, fn, idx, dma
        self.dmaval = 0
        self.deps = []
        self.signal = False
        self.sigval = 0


class _DmaSem:
    def __init__(self, sem):
        self.sem = sem
        self.count = 0


class Prog:
    def __init__(self):
        self.ops = {e: [] for e in ENGS}
        self.bufs = {}

    def _buf(self, k):
        b = self.bufs.get(k)
        if b is None:
            b = self.bufs[k] = _Buf(isinstance(k, tuple))
        return b

    def add(self, eng, fn, r=(), w=(), dma=None):
        op = _Op(eng, fn, len(self.ops[eng]), dma)
        if dma is not None:
            dma.count += 16
            op.dmaval = dma.count
        deps = set()
        for k in r:
            b = self._buf(k)
            if b.w is not None:
                deps.add(b.w)
            if b.psum:
                for o in b.rs:
                    if o.eng != eng:
                        deps.add(o)
        for k in w:
            b = self._buf(k)
            if b.w is not None:
                deps.add(b.w)
            deps.update(b.rs)
        for d in deps:
            if d is op:
                continue
            if d.dma is not None or op.dma is not None or d.eng != eng:
                need = True
            elif eng == "pe":
                need = False
            else:
                need = (op.idx - d.idx) <= 2
            if need:
                op.deps.append(d)
                if d.dma is None:
                    d.signal = True
        for k in r:
            self._buf(k).rs.append(op)
        for k in w:
            b = self._buf(k)
            b.w = op
            b.rs = []
        self.ops[eng].append(op)
        return op

    def emit(self, nc, engsem):
        for e in ENGS:
            n = 0
            for op in self.ops[e]:
                if op.signal:
                    n += 1
                    op.sigval = n
        handles = {"pe": "tensor", "act": "scalar", "dve": "vector", "pool": "gpsimd", "sp": "sync"}
        with nc.Block() as block:
            for e in ENGS:
                ops = self.ops[e]

                def body(h, ops=ops, e=e):
                    waited = {}
                    for op in ops:
                        req = {}
                        for d in op.deps:
                            if d.dma is not None:
                                s, v = d.dma.sem, d.dmaval
                            else:
                                s, v = engsem[d.eng], d.sigval
                            key = id(s)
                            if key not in req or req[key][1] < v:
                                req[key] = (s, v)
                        for key, (s, v) in req.items():
                            if waited.get(key, 0) < v:
                                h.wait_ge(s, v)
                                waited[key] = v
                        if op.fn is None:
                            continue
                        ins = op.fn(h)
                        if op.dma is not None:
                            ins.then_inc(op.dma.sem, 16)
                        elif op.signal:
                            ins.then_inc(engsem[e], 1)

                getattr(block, handles[e])(body)


class TT:
    def __init__(self, arena, off, shape, dt, parts=128):
        self.off, self.shape, self.dt = off, tuple(shape), dt
        self.es = 2 if dt == BF16 else 4
        n = 1
        for s in shape:
            n *= s
        self.nbytes = n * self.es
        assert off % 4 == 0
        base = arena[0:parts, off // 2:(off + self.nbytes) // 2]
        if dt == F32:
            base = base.bitcast(F32)
        if len(shape) == 2:
            base = base.rearrange("p (a b) -> p a b", b=shape[1])
        elif len(shape) == 3:
            base = base.rearrange("p (a b c) -> p a b c", b=shape[1], c=shape[2])
        self.ap = base
        self.all = list(range(off // PG, (off + self.nbytes + PG - 1) // PG))

    def ch(self, i, n=1):
        cb = self.nbytes // self.shape[0]
        lo = self.off + i * cb
        hi = lo + n * cb
        return list(range(lo // PG, (hi + PG - 1) // PG))


def build_nc(NT=2 * TPS):
    nc = bass.Bass("TRN2", target_bir_lowering=False)
    x_d = nc.dram_tensor("x", [2, S, D], F32, kind="ExternalInput").ap()
    mem_d = nc.dram_tensor("mem", [2, 256, D], F32, kind="ExternalInput").ap()
    w_d = nc.dram_tensor("w_all", [D, NG * 512], F32, kind="ExternalInput").ap()
    cb_d = nc.dram_tensor("cb", [128, C_END], F32, kind="ExternalInput").ap()
    pf_d = nc.dram_tensor("pf", [128, PF_END], F32, kind="ExternalInput").ap()
    brow_d = nc.dram_tensor("brow", [1, 1536], F32, kind="ExternalInput").ap()
    wup_d = nc.dram_tensor("wup", [17, 512], F32, kind="ExternalInput").ap()
    out_d = nc.dram_tensor("out", [2, S, D], F32, kind="ExternalOutput").ap()

    es = ExitStack()
    with es:
        ARENA = 207 * 1024
        arena = es.enter_context(nc.sbuf_tensor("arena", [128, ARENA // 2], BF16))
        ps = [es.enter_context(nc.psum_tensor(f"ps{i}", [128, 512], F32)) for i in range(8)]
        engsem = {e: es.enter_context(nc.semaphore(f"s_{e}")) for e in ENGS}

        def dsem(name):
            return _DmaSem(es.enter_context(nc.semaphore(name)))

        wsem = [dsem(f"w{i}") for i in range(3)]
        csem, csem2, xsem, msem = dsem("cst"), dsem("cst2"), dsem("xld"), dsem("mld")
        osem = [dsem(f"o{i}") for i in range(4)]
        xrsem = [dsem(f"xr{i}") for i in range(4)]

        P = Prog()
        cur = [0]

        def al(shape, dt, at=None, parts=128):
            if at is None:
                nb_ = (2 if dt == BF16 else 4)
                for s_ in shape:
                    nb_ *= s_
                algn = PG if nb_ >= PG else 64
                off = (cur[0] + algn - 1) // algn * algn
                t = TT(arena, off, shape, dt, parts)
                cur[0] = off + t.nbytes
            else:
                t = TT(arena, at, shape, dt, parts)
            assert t.off + t.nbytes <= ARENA, (t.off, t.nbytes)
            _ALLOCS.append((t.off, t.nbytes, shape, at))
            return t

        cb = al([C_END], BF16)
        pf = al([PF_END], F32)
        brow = al([1536], BF16, parts=1)
        wup = al([512], BF16, parts=17)
        bhalf = al([24], F32)
        esink = al([16], F32)
        wbuf = [al([8, 512], BF16) for _ in range(3)]
        x_tm = al([4, 1024], F32)
        hT = al([8, 512], BF16)
        junk = al([1024], BF16)
        xn = al([1024], BF16)
        ss = al([12], F32)
        lnss = al([12], F32)
        rstd_x = al([12], F32)
        zact = al([8, 512], BF16)
        qbuf = al([8, 512], BF16)
        bact = al([8, 512], BF16)
        tg = al([8, 512], BF16)
        v_tm = al([4, 1024], BF16)
        krot = al([4, 512], BF16)
        khi = al([4, 512], BF16)
        kprev = al([4, 128], BF16)
        kprev_hi = al([4, 128], BF16)
        sv = al([5, 4, 66], BF16)
        _OFFS.update(krot=krot.off, kprev=kprev.off, sv=sv.off)
        glr = al([512], BF16, parts=17)
        macc = al([8, 512], F32)
        mtmp = [al([512], F32) for _ in range(2)]
        xres = [al([1024], F32, at=macc.off + i * 4096) for i in range(4)]
        outt = xres
        mkT = al([8, 256], BF16)
        mv = al([2, 1024], BF16)
        memnT = al([8, 256], BF16)
        scr0 = cur[0] = (cur[0] + PG - 1) // PG * PG
        nla = al([4, 512], BF16)
        etmp = al([512], F32)
        eb = al([4, 4, 128], BF16)
        einv = al([4, 4, 128], BF16)
        dec = al([4, 4], F32)
        qdec = al([4, 512], BF16)
        kinv = al([4, 512], BF16)
        attm = al([4, 128], BF16)
        kinv_tm = al([4, 128], BF16)
        sq = [al([4, 128], BF16) for _ in range(2)]
        lnv = al([4, 128], F32)
        rstd = al([4, 128], F32)
        tgl = [al([4, 128], BF16) for _ in range(2)]
        kdec = al([4, 128], BF16)
        S_f = al([4, 256], F32)
        S_b = al([4, 256], BF16)
        _OFFS.update(S_f=S_f.off, S_b=S_b.off)
        scr_end = cur[0]
        cur[0] = scr0
        xs = [al([512], BF16) for _ in range(2)]
        t1 = [al([512], F32) for _ in range(2)]
        ur = [al([512], F32) for _ in range(2)]
        Pt = [al([4, 128], BF16) for _ in range(4)]
        o_tm = al([1024], BF16)
        den4 = al([16], F32)
        rec4 = al([16], F32)
        assert cur[0] <= S_f.off, (cur[0], S_f.off)
        cur[0] = scr0
        mem_tm = al([2, 1024], F32)
        Px = [al([512], BF16) for _ in range(2)]
        recx = al([512], F32)
        wx = [al([512], F32) for _ in range(2)]
        assert cur[0] <= S_f.off, (cur[0], S_f.off)
        cur[0] = scr_end
        dbg = junk
        _OFFS.update(dbg=dbg.off, end=cur[0])

        cbv = cb.ap
        ident = cbv[:, C_ID:C_ID + 128]
        ones = cbv[:, C_ONE:C_ONE + 128]
        permM = cbv[:, C_PERM:C_PERM + 128]
        tri = cbv[:, C_TRI:C_TRI + 128]
        tri4 = cbv[:, C_TRI:C_TRI + 512].rearrange("p (a b) -> p a b", b=128)
        negp = cbv[:, C_NTRI:C_NTRI + 512]
        negc = cbv[:, C_NEGC:C_NEGC + 512]
        pfv = pf.ap
        gmx = pfv[:, PF_GMX:PF_GMX + 1024].rearrange("p (a b) -> p a b", b=128)
        gmemx = pfv[:, PF_GMEM:PF_GMEM + 1024].rearrange("p (a b) -> p a b", b=128)
        gfin = pfv[:, PF_GFIN:PF_GFIN + 1024]

        bank = [0]

        def nb():
            b = bank[0]
            bank[0] = (b + 1) % 8
            return b

        def PS(b):
            return ("ps", b)

        def dma(q, out, in_, w, r, sem):
            P.add(q, lambda h, out=out, in_=in_: h.dma_start(out=out, in_=in_), r=r, w=w, dma=sem)

        dma("pool", cb.ap, cb_d, cb.all, [], csem)
        dma("sp", pf.ap, pf_d, pf.all, [], csem2)
        dma("pool", brow.ap, brow_d, brow.all, [], dsem("cst3"))
        dma("pool", wup.ap, wup_d, wup.all, [], dsem("cst4"))
        P.add("dve", lambda h: h.tensor_scalar(out=bhalf.ap, in0=pfv[:, PF_BIAS + 52:PF_BIAS + 76], scalar1=0.5,
                                               scalar2=None, op0=ALU.mult), r=pf.all, w=bhalf.all)
        P.add("act", lambda h: h.activation(out=esink.ap, in_=pfv[:, PF_SINK:PF_SINK + 16], func=AF.Exp),
              r=pf.all, w=esink.all)
        P.add("pool", lambda h: h.memset(sv.ap, 1.0), w=sv.all)
        P.add("pool", lambda h: h.memset(glr.ap, 1.0), w=glr.all)
        for z_ in (krot, khi, kprev, kprev_hi):
            P.add("pool", lambda h, z_=z_: h.memset(z_.ap, 0.0), w=z_.all)

        wseq = []
        MEMG = [G_MK, G_MK + 1, G_MV, G_MV + 1]
        for t in range(NT):
            if t == 0:
                wseq += MEMG
            wseq += [G_SV, G_GQ, G_GK, G_GZ, G_GZ + 1, G_GV, G_GV + 1, G_GT, G_GT + 1, G_BRA, G_BRA + 1]
            wseq += [G_SQ, G_SQ + 1, G_SK, G_SZ, G_SZ + 1, G_GT + 2, G_GT + 3, G_BRB, G_BRB + 1]
            wseq += [G_XQ, G_XQ + 1, G_XZ, G_XZ + 1, G_GT + 4, G_GT + 5]
            if t + 1 < NT and (t + 1) % TPS == 0:
                wseq += MEMG
            wseq += [G_BRC, G_BRC + 1, G_OUT, G_OUT + 1]
        wstate = {"loaded": 0, "used": 0}

        def w_load_upto(n):
            while wstate["loaded"] < min(n, len(wseq)):
                u = wstate["loaded"]
                g = wseq[u]
                slot = u % 3
                src = w_d[:, g * 512:(g + 1) * 512].rearrange("(k p) c -> p k c", p=128)
                dma("pool", wbuf[slot].ap, src, wbuf[slot].all, [], wsem[slot])
                wstate["loaded"] += 1

        def use_w(g, ahead=3):
            u = wstate["used"]
            assert wseq[u] == g, (u, wseq[u], g)
            w_load_upto(u + ahead)
            wstate["used"] += 1
            return wbuf[u % 3]

        def fm_group_gen(g, src, evac, n=512, nj=4, bank=None):
            wb = use_w(g)
            for j in range(nj):
                b = nb() if bank is None else bank

                def f(h, wb=wb, j=j, b=b):
                    for k in range(8):
                        ins = h.matmul(ps[b][:, 0:n], wb.ap[:, k, j * 128:(j + 1) * 128], src.ap[:, k, 0:n],
                                       start=(k == 0), stop=(k == 7))
                    return ins

                P.add("pe", f, r=wb.all + src.all, w=[PS(b)])
                evac(j, b)
                yield

        def fm_group(*a, **k):
            for _ in fm_group_gen(*a, **k):
                pass

        def tm_group(g, src, evac, ntb=4, ncol=512, bias_col=None):
            wb = use_w(g)
            for tb in range(ntb):
                b = nb()

                def f(h, wb=wb, tb=tb, b=b):
                    for k in range(8):
                        ins = h.matmul(ps[b][:, 0:ncol], src.ap[:, k, tb * 128:(tb + 1) * 128], wb.ap[:, k, 0:ncol],
                                       start=(k == 0), stop=(k == 7 and bias_col is None))
                    if bias_col is not None:
                        ins = h.matmul(ps[b][:, 0:ncol], cbv[0:1, C_ONE:C_ONE + 128],
                                       brow.ap[0:1, bias_col:bias_col + ncol], start=False, stop=True)
                    return ins

                P.add("pe", f, r=wb.all + src.all + cb.all + brow.all, w=[PS(b)])
                evac(tb, b)
            return wb

        def bias(c):
            return pfv[:, PF_BIAS + c:PF_BIAS + c + 1]

        def act_evac(dst_t, func, gbase, scale=1.0, bias_t=None):
            def ev(j, b, c0=0):
                c = c0 + j
                bap = bias(gbase * 4 + j) if bias_t is None else bias_t(j)
                P.add("act", lambda h, c=c, b=b, bap=bap: h.activation(out=dst_t.ap[:, c, :], in_=ps[b][:, :], func=func,
                                                                        bias=bap, scale=scale),
                      r=[PS(b)] + pf.all + bhalf.all, w=dst_t.ch(c))
            return ev

        def norm_transpose(src_t, nblk, gx, dstT, dcols, phase="ab", sc=0, prescale=False):
            nj_ = 1024 // dcols
            if "a" in phase:
                for tb in range(nblk):
                    if phase == "a":
                        jout, jw = o_tm.ap, o_tm.all
                        jin = src_t.ap[:, tb, :]
                    else:
                        jout, jw = dstT.ap[:, 0:nj_, :], dstT.ch(0, nj_)
                        jin = src_t.ap[:, tb, :].rearrange("p (a b) -> p a b", b=dcols)
                    P.add("act", lambda h, tb=tb, jout=jout, jin=jin: h.activation(
                        out=jout, in_=jin, func=AF.Square, accum_out=ss.ap[:, sc + tb:sc + tb + 1]),
                          r=src_t.ch(tb), w=jw + ss.all)
                P.add("act", lambda h: h.activation(out=lnss.ap[:, sc:sc + nblk], in_=ss.ap[:, sc:sc + nblk], func=AF.Ln,
                                                    bias=EPS, scale=1.0 / 1024.0), r=ss.all, w=lnss.all)
                P.add("act", lambda h: h.activation(out=rstd_x.ap[:, sc:sc + nblk], in_=lnss.ap[:, sc:sc + nblk],
                                                    func=AF.Exp, scale=-0.5), r=lnss.all, w=rstd_x.all)
            def scale_blk(tb):
                xb = (xn, junk)[tb % 2]
                P.add("dve", lambda h, tb=tb, xb=xb: h.tensor_scalar(out=xb.ap, in0=src_t.ap[:, tb, :],
                                                                     scalar1=rstd_x.ap[:, sc + tb:sc + tb + 1],
                                                                     scalar2=None, op0=ALU.mult),
                      r=src_t.ch(tb) + rstd_x.all, w=xb.all)
            if phase == "a":
                if prescale:
                    scale_blk(0)
                    scale_blk(1)
                return
            for tb in range(nblk):
                xb = (xn, junk)[tb % 2]
                if not (phase == "b" and prescale and tb < 2):
                    scale_blk(tb)
                for half in range(2):
                    b = nb()

                    def f(h, b=b, half=half, xb=xb):
                        for c in range(4):
                            k = half * 4 + c
                            ins = h.matmul(ps[b][:, c * 128:(c + 1) * 128], xb.ap[:, k * 128:(k + 1) * 128], ident,
                                           start=True, stop=True)
                        return ins

                    P.add("pe", f, r=xb.all + cb.all, w=[PS(b)])
                    P.add("dve", lambda h, b=b, half=half, tb=tb: h.tensor_tensor(
                        out=dstT.ap[:, half * 4:half * 4 + 4, tb * 128:(tb + 1) * 128],
                        in0=ps[b][:, :].rearrange("p (a b) -> p a b", b=128),
                        in1=gx[:, half * 4:half * 4 + 4, :], op=ALU.mult),
                          r=[PS(b)] + pf.all, w=dstT.ch(half * 4, 4))

        def gates_gen(bi, bank=None):
            for half in range(2):
                gg = G_GT + 2 * bi + half

                def ev(j, b, half=half, gg=gg):
                    c = half * 4 + j
                    P.add("act", lambda h, c=c, b=b, gg=gg, j=j: h.activation(
                        out=tg.ap[:, c, :], in_=ps[b][:, :], func=AF.Tanh,
                        bias=bhalf.ap[:, (gg - G_GT) * 4 + j:(gg - G_GT) * 4 + j + 1], scale=0.5),
                          r=[PS(b)] + bhalf.all, w=tg.ch(c))
                yield from fm_group_gen(gg, hT, ev, bank=bank)

        def branch_mm(bi, gbr):
            for half in range(2):
                def ev(j, b, half=half):
                    c = half * 4 + j
                    if bi == 0:
                        P.add("dve", lambda h, c=c, b=b: h.scalar_tensor_tensor(
                            out=macc.ap[:, c, :], in0=tg.ap[:, c, :], scalar=1.0, in1=ps[b][:, :],
                            op0=ALU.add, op1=ALU.mult), r=[PS(b)] + tg.ch(c), w=macc.ch(c))
                    else:
                        mt = mtmp[c % 2]
                        P.add("dve", lambda h, c=c, b=b, mt=mt: h.scalar_tensor_tensor(
                            out=mt.ap, in0=tg.ap[:, c, :], scalar=1.0, in1=ps[b][:, :],
                            op0=ALU.add, op1=ALU.mult), r=[PS(b)] + tg.ch(c), w=mt.all)
                        if bi == 1:
                            P.add("pool", lambda h, c=c, mt=mt: h.tensor_tensor(
                                out=macc.ap[:, c, :], in0=macc.ap[:, c, :], in1=mt.ap, op=ALU.add),
                                  r=mt.all + macc.ch(c), w=macc.ch(c))
                        else:
                            P.add("pool", lambda h, c=c, mt=mt: h.tensor_tensor(
                                out=qbuf.ap[:, c, :], in0=macc.ap[:, c, :], in1=mt.ap, op=ALU.add),
                                  r=mt.all + macc.ch(c), w=qbuf.ch(c))
                fm_group(gbr + half, bact, ev)

        def load_x(t):
            sq_i, tok0 = t // TPS, (t % TPS) * T
            dma("sp", x_tm.ap, x_d[sq_i, tok0:tok0 + T, :].rearrange("(tb p) d -> p tb d", p=128), x_tm.all, [], xsem)

        def prep_tile(t, stats_done=False):
            sq_i = t // TPS
            if t % TPS == 0:
                dma("sp", mem_tm.ap, mem_d[sq_i].rearrange("(mb p) d -> p mb d", p=128), mem_tm.all, [], msem)
                norm_transpose(mem_tm, 2, gmemx, memnT, 256, sc=8)
                for half in range(2):
                    def ev(j, b, half=half):
                        c = half * 4 + j
                        P.add("act", lambda h, c=c, b=b: h.copy(out=mkT.ap[:, c, :], in_=ps[b][:, 0:256]),
                              r=[PS(b)], w=mkT.ch(c))
                    fm_group(G_MK + half, memnT, ev, n=256)
                for half in range(2):
                    def ev(mb, b, half=half):
                        P.add("dve", lambda h, mb=mb, b=b: h.tensor_copy(out=mv.ap[:, mb, half * 512:(half + 1) * 512],
                                                                         in_=ps[b][:, :]),
                              r=[PS(b)], w=mv.ch(mb))
                    tm_group(G_MV + half, memnT, ev, ntb=2)
                P.add("pool", lambda h: h.memset(S_f.ap, 0.0), w=S_f.all)
                P.add("pool", lambda h: h.memset(S_b.ap, 0.0), w=S_b.all)
            norm_transpose(x_tm, 4, gmx, hT, 512, phase="b" if stats_done else "ab", prescale=(t % TPS != 0))
            if t + 1 < NT:
                load_x(t + 1)

        for t in range(NT):
            sq_i = t // TPS
            ti = t % TPS
            tok0 = ti * T

            if t == 0:
                load_x(0)
                prep_tile(0)
            rope_pend = []

            def rope_flush():
                while rope_pend:
                    rope_pend.pop(0)()

            def rope_evac(dst, gbase, pbank=None):
                def ev(j, b, c0=0):
                    c = c0 + j
                    x_ = xs[c % 2]
                    t_ = t1[c % 2]
                    u_ = ur[c % 2]
                    cosap = cbv[:, C_COS + tok0:C_COS + tok0 + T]
                    sinap = cbv[:, C_SIN + tok0:C_SIN + tok0 + T]
                    P.add("act", lambda h, b=b, x_=x_, j=j: h.activation(out=x_.ap, in_=ps[b][:, :], func=AF.Identity,
                                                                        bias=bias(gbase * 4 + j), scale=1.0),
                          r=[PS(b)] + pf.all, w=x_.all)
                    P.add("dve", lambda h, b=b, t_=t_, cosap=cosap, j=j: h.scalar_tensor_tensor(
                        out=t_.ap, in0=ps[b][:, :], scalar=bias(gbase * 4 + j), in1=cosap, op0=ALU.add, op1=ALU.mult),
                          r=[PS(b)] + pf.all + cb.all, w=t_.all)

                    def tail(c=c, x_=x_, t_=t_, u_=u_, sinap=sinap):
                        b2 = nb() if pbank is None else pbank
                        P.add("pe", lambda h, b2=b2, x_=x_: h.matmul(ps[b2][:, :], permM, x_.ap, start=True, stop=True),
                              r=x_.all + cb.all, w=[PS(b2)])
                        P.add("dve", lambda h, b2=b2, u_=u_, sinap=sinap: h.tensor_tensor(
                            out=u_.ap, in0=ps[b2][:, :], in1=sinap, op=ALU.mult),
                              r=[PS(b2)] + cb.all, w=u_.all)
                        if dst is krot:
                            P.add("pool", lambda h, c=c, t_=t_, u_=u_: h.tensor_tensor(
                                out=krot.ap[0:64, c, :], in0=t_.ap[0:64, :], in1=u_.ap[0:64, :], op=ALU.add),
                                  r=t_.all + u_.all, w=krot.ch(c))
                            P.add("pool", lambda h, c=c, t_=t_, u_=u_: h.tensor_tensor(
                                out=khi.ap[64:128, c, :], in0=t_.ap[64:128, :], in1=u_.ap[64:128, :], op=ALU.add),
                                  r=t_.all + u_.all, w=khi.ch(c))
                        else:
                            P.add("pool", lambda h, c=c, t_=t_, u_=u_: h.tensor_tensor(out=dst.ap[:, c, :], in0=t_.ap,
                                                                                      in1=u_.ap, op=ALU.add),
                                  r=t_.all + u_.all, w=dst.ch(c))
                    prev = list(rope_pend)
                    del rope_pend[:]
                    rope_pend.append(tail)
                    for f_ in prev:
                        f_()
                return ev

            if _SNAP == 0 and t == 1:
                P.add('pool', lambda h: h.tensor_copy(out=dbg.ap[:, 0:512], in_=kprev.ap.rearrange('p a b -> p (a b)')), r=kprev.all, w=dbg.all)
            if _STOP == 2:
                break
            qraw, kraw = qbuf, qbuf

            def ev_sv(tb, b):
                P.add("dve", lambda h, tb=tb, b=b: h.tensor_copy(
                    out=sv.ap[:, tb + 1, :, 0:64], in_=ps[b][:, 0:256].rearrange("p (g d) -> p g d", d=64)),
                      r=[PS(b)], w=sv.ch(tb + 1))
            wb = tm_group(G_SV, hT, ev_sv, ncol=256, bias_col=1024)
            b = nb()

            def f(h, wb=wb, b=b):
                for k in range(8):
                    ins = h.matmul(ps[b][0:16, :], wb.ap[:, k, 256:272], hT.ap[:, k, :], start=(k == 0), stop=(k == 7))
                return ins
            P.add("pe", f, r=wb.all + hT.all, w=[PS(b)])
            P.add("act", lambda h, b=b: h.activation(out=glr.ap[0:16, :], in_=ps[b][0:16, :], func=AF.Identity,
                                                     bias=pfv[0:16, PF_GLR:PF_GLR + 1], scale=1.0),
                  r=[PS(b)] + pf.all, w=glr.all)

            def s1_z(tb):
                cs = slice(tb * 128, (tb + 1) * 128)
                b = nb()
                P.add("pe", lambda h, b=b, cs=cs: h.matmul(ps[b][:, :], glr.ap[0:17, cs], wup.ap[0:17, :],
                                                           start=True, stop=True),
                      r=glr.all + wup.all, w=[PS(b)])
                P.add("act", lambda h, b=b: h.activation(out=etmp.ap, in_=ps[b][:, :], func=AF.Exp, scale=-1.0),
                      r=[PS(b)], w=etmp.all)
                P.add("act", lambda h, tb=tb: h.activation(out=nla.ap[:, tb, :], in_=etmp.ap, func=AF.Ln, bias=1.0,
                                                           scale=1.0),
                      r=etmp.all, w=nla.ch(tb))

            def s1_c(tb):
                b = nb()

                def f(h, b=b, tb=tb):
                    for hh in range(4):
                        ins = h.matmul(ps[b][:, hh * 128:(hh + 1) * 128], nla.ap[:, tb, hh * 128:(hh + 1) * 128], tri,
                                       start=True, stop=True)
                    return ins
                P.add("pe", f, r=nla.ch(tb) + cb.all, w=[PS(b)])
                pb = ps[b][:, :].rearrange("p (a b) -> p a b", b=128)
                P.add("act", lambda h, pb=pb, tb=tb: h.activation(out=eb.ap[:, tb, :, :], in_=pb, func=AF.Exp,
                                                                  scale=-1.0 / 16.0, bias=-0.5 * math.log(128.0)),
                      r=[PS(b)], w=eb.ch(tb))
                P.add("act", lambda h, pb=pb, tb=tb: h.activation(out=einv.ap[:, tb, :, :], in_=pb, func=AF.Exp,
                                                                  scale=1.0 / 16.0),
                      r=[PS(b)], w=einv.ch(tb))
                P.add("act", lambda h, pb=pb, tb=tb: h.activation(out=dec.ap[:, tb, :], in_=pb[:, :, 127], func=AF.Exp,
                                                                  scale=-1.0 / 16.0),
                      r=[PS(b)], w=dec.all)

            fm_group(G_GQ, hT, lambda j, b: act_evac(qbuf, AF.Identity, G_GQ)(j, b, 0))
            for tb in range(4):
                s1_z(tb)
            fm_group(G_GK, hT, lambda j, b: act_evac(qbuf, AF.Identity, G_GK)(j, b, 4))
            s1_c(0)
            s1_c(1)
            fm_group(G_GZ, hT, lambda j, b: act_evac(zact, AF.Silu, G_GZ)(j, b, 0))
            s1_c(2)
            s1_c(3)
            fm_group(G_GZ + 1, hT, lambda j, b: act_evac(zact, AF.Silu, G_GZ + 1)(j, b, 4))
            for half in range(2):
                def ev(tb, b, half=half):
                    P.add("dve", lambda h, tb=tb, b=b: h.tensor_copy(out=v_tm.ap[:, tb, half * 512:(half + 1) * 512],
                                                                     in_=ps[b][:, :]),
                          r=[PS(b)], w=v_tm.ch(tb))
                tm_group(G_GV + half, hT, ev, bias_col=half * 512)

            if _STOP == 3:
                break
            gg0 = gates_gen(0, bank=7)
            for tb in range(4):
                cs = slice(tb * 128, (tb + 1) * 128)
                P.add("dve", lambda h, cs=cs, tb=tb: h.tensor_tensor(out=qdec.ap[:, :, cs], in0=qbuf.ap[:, 0:4, cs],
                                                                     in1=eb.ap[:, tb, :, :], op=ALU.mult),
                      r=qbuf.ch(0, 4) + eb.ch(tb), w=qdec.all)
                P.add("pool", lambda h, cs=cs, tb=tb: h.tensor_tensor(out=kinv.ap[:, :, cs], in0=qbuf.ap[:, 4:8, cs],
                                                                      in1=einv.ap[:, tb, :, :], op=ALU.mult),
                      r=qbuf.ch(4, 4) + einv.ch(tb), w=kinv.all)

            B_ATT, B_KT, B_O, B_SS, B_S = 0, 1, (2, 3), 4, (5, 6)

            def gla_norm_tail(tb, cs):
                def f(h):
                    for hh in range(4):
                        o = ps[B_SS][:, hh * 128:(hh + 1) * 128]
                        h.matmul(o, ones, sq[0].ap[:, hh, :], start=True, stop=False)
                        ins = h.matmul(o, ones, sq[1].ap[:, hh, :], start=False, stop=True)
                    return ins
                P.add("pe", f, r=sq[0].all + sq[1].all + cb.all, w=[PS(B_SS)])
                P.add("act", lambda h: h.activation(out=lnv.ap,
                                                    in_=ps[B_SS][:, :].rearrange("p (a b) -> p a b", b=128),
                                                    func=AF.Ln, bias=EPS, scale=1.0 / 256.0),
                      r=[PS(B_SS)], w=lnv.all)
                P.add("act", lambda h: h.activation(out=rstd.ap, in_=lnv.ap, func=AF.Exp, scale=-0.5),
                      r=lnv.all, w=rstd.all)
                for vc in range(2):
                    P.add("dve", lambda h, vc=vc: h.scalar_tensor_tensor(
                        out=tgl[vc].ap, in0=ps[B_O[vc]][:, :].rearrange("p (a b) -> p a b", b=128),
                        scalar=pfv[:, PF_GLA + vc:PF_GLA + vc + 1], in1=rstd.ap, op0=ALU.mult, op1=ALU.mult),
                          r=[PS(B_O[vc])] + pf.all + rstd.all, w=tgl[vc].all)
                    P.add("pool", lambda h, vc=vc, cs=cs: h.tensor_tensor(
                        out=bact.ap[:, vc::2, cs], in0=tgl[vc].ap, in1=zact.ap[:, vc::2, cs], op=ALU.mult),
                          r=tgl[vc].all + zact.all, w=bact.all)

            def g_att(tb):
                cs = slice(tb * 128, (tb + 1) * 128)

                def f(h, cs=cs):
                    for hh in range(4):
                        ins = h.matmul(ps[B_ATT][:, hh * 128:(hh + 1) * 128], kinv.ap[:, hh, cs], qdec.ap[:, hh, cs],
                                       start=True, stop=True)
                    return ins
                P.add("pe", f, r=kinv.all + qdec.all, w=[PS(B_ATT)])
                P.add("dve", lambda h: h.tensor_tensor(out=attm.ap,
                                                       in0=ps[B_ATT][:, :].rearrange("p (a b) -> p a b", b=128),
                                                       in1=tri4, op=ALU.mult),
                      r=[PS(B_ATT)] + cb.all, w=attm.all)
                P.add("dve", lambda h, cs=cs, tb=tb: h.tensor_tensor(
                    out=kdec.ap, in0=kinv.ap[:, :, cs], in1=dec.ap[:, tb, :].unsqueeze(2).to_broadcast([128, 4, 128]),
                    op=ALU.mult), r=kinv.all + dec.all, w=kdec.all)

            def g_kT(tb):
                def f(h):
                    for hh in range(4):
                        ins = h.matmul(ps[B_KT][:, hh * 128:(hh + 1) * 128], kdec.ap[:, hh, :], ident,
                                       start=True, stop=True)
                    return ins
                P.add("pe", f, r=kdec.all + cb.all, w=[PS(B_KT)])
                P.add("act", lambda h: h.copy(out=kinv_tm.ap,
                                              in_=ps[B_KT][:, :].rearrange("p (a b) -> p a b", b=128)),
                      r=[PS(B_KT)], w=kinv_tm.all)

            def g_smm(tb):
                for hp in range(2):
                    def f(h, hp=hp, tb=tb):
                        for q in range(2):
                            hh = hp * 2 + q
                            ins = h.matmul(ps[B_S[hp]][:, q * 256:(q + 1) * 256], kinv_tm.ap[:, hh, :],
                                           v_tm.ap[:, tb, hh * 256:(hh + 1) * 256], start=True, stop=True)
                        return ins
                    P.add("pe", f, r=kinv_tm.all + v_tm.ch(tb), w=[PS(B_S[hp])])

            def g_o(tb):
                cs = slice(tb * 128, (tb + 1) * 128)
                for vc in range(2):
                    def f(h, vc=vc, tb=tb, cs=cs):
                        for hh in range(4):
                            o = ps[B_O[vc]][:, hh * 128:(hh + 1) * 128]
                            vcol = hh * 256 + vc * 128
                            h.matmul(o, v_tm.ap[:, tb, vcol:vcol + 128], attm.ap[:, hh, :], start=True, stop=False)
                            ins = h.matmul(o, S_b.ap[:, hh, vc * 128:(vc + 1) * 128], qdec.ap[:, hh, cs],
                                           start=False, stop=True)
                        return ins
                    P.add("pe", f, r=v_tm.ch(tb) + attm.all + S_b.all + qdec.all, w=[PS(B_O[vc])])
                    P.add("act", lambda h, vc=vc: h.activation(
                        out=sq[vc].ap, in_=ps[B_O[vc]][:, :].rearrange("p (a b) -> p a b", b=128), func=AF.Square),
                          r=[PS(B_O[vc])], w=sq[vc].all)

            def g_supd(tb):
                for hh in range(4):
                    hp, q = hh // 2, hh % 2
                    P.add("dve", lambda h, hh=hh, hp=hp, q=q, tb=tb: h.scalar_tensor_tensor(
                        out=S_f.ap[:, hh, :], in0=S_f.ap[:, hh, :], scalar=dec.ap[:, tb, hh:hh + 1],
                        in1=ps[B_S[hp]][:, q * 256:(q + 1) * 256], op0=ALU.mult, op1=ALU.add),
                          r=[PS(B_S[hp])] + S_f.ch(hh) + dec.all, w=S_f.ch(hh))
                    P.add("pool", lambda h, hh=hh: h.tensor_copy(out=S_b.ap[:, hh, :], in_=S_f.ap[:, hh, :]),
                          r=S_f.ch(hh), w=S_b.ch(hh))

            g_att(0)
            g_kT(0)
            g_smm(0)
            for tb in range(4):
                cs = slice(tb * 128, (tb + 1) * 128)
                g_o(tb)
                if tb + 1 < 4:
                    g_att(tb + 1)
                g_supd(tb)
                next(gg0, None)
                if tb + 1 < 4:
                    g_kT(tb + 1)
                gla_norm_tail(tb, cs)
                if tb + 1 < 4:
                    g_smm(tb + 1)
                next(gg0, None)

            if _SNAP == 1 and t == 1:
                P.add('pool', lambda h: h.tensor_copy(out=dbg.ap[:, 0:512], in_=kprev.ap.rearrange('p a b -> p (a b)')), r=kprev.all, w=dbg.all)
            if _STOP == 4:
                break
            for _ in gg0:
                pass
            branch_mm(0, G_BRA)
            if _SNAP == 2 and t == 1:
                P.add('pool', lambda h: h.tensor_copy(out=dbg.ap[:, 0:512], in_=kprev.ap.rearrange('p a b -> p (a b)')), r=kprev.all, w=dbg.all)

            if _STOP == 5:
                break
            fm_group(G_SQ, hT, lambda j, b: rope_evac(qbuf, G_SQ)(j, b, 0))
            fm_group(G_SQ + 1, hT, lambda j, b: rope_evac(qbuf, G_SQ + 1)(j, b, 4))
            fm_group(G_SK, hT, lambda j, b: rope_evac(krot, G_SK)(j, b, 0))
            rope_flush()
            if _SNAP == 3 and t == 1:
                P.add('pool', lambda h: h.tensor_copy(out=dbg.ap[:, 0:512], in_=kprev.ap.rearrange('p a b -> p (a b)')), r=kprev.all, w=dbg.all)

            if _STOP == 6:
                break
            gg1 = gates_gen(1, bank=7)
            SB_SC, SB_PV, SB_TR = ((0, 1), (2, 3)), (4, 5), 6

            def swa_A(tb, g):
                cs = slice(tb * 128, (tb + 1) * 128)
                has_prev = not (ti == 0 and tb == 0)
                kbs = (["prev"] if has_prev else []) + ["cur"]
                pts = {}
                for kb in kbs:
                    b = SB_SC[g % 2][0 if kb == "prev" else 1]
                    pt = Pt[(g % 2) * 2 + (0 if kb == "prev" else 1)]
                    pts[kb] = pt
                    if kb == "cur":
                        ksrc = (lambda p0, g=g, cs=cs: (krot if p0 == 0 else khi).ap[:, g, cs])
                        kr = krot.ch(g) + khi.ch(g)
                    elif tb > 0:
                        pc = slice((tb - 1) * 128, tb * 128)
                        ksrc = (lambda p0, g=g, pc=pc: (krot if p0 == 0 else khi).ap[:, g, pc])
                        kr = krot.ch(g) + khi.ch(g)
                    else:
                        ksrc = (lambda p0, g=g: (kprev if p0 == 0 else kprev_hi).ap[:, g, :])
                        kr = kprev.all + kprev_hi.all

                    negm = negc if kb == "cur" else negp

                    def f(h, b=b, g=g, ksrc=ksrc, cs=cs, negm=negm):
                        h.matmul(ps[b][:, :], ident, negm, start=True, stop=False)
                        for j in range(4):
                            qh = 4 * g + j
                            p0 = (qh % 2) * 64
                            ins = h.matmul(ps[b][:, j * 128:(j + 1) * 128], ksrc(p0),
                                           qbuf.ap[:, qh // 2, cs], start=False, stop=(j == 3))
                        return ins
                    P.add("pe", f, r=kr + qbuf.ch(2 * g, 2) + cb.all, w=[PS(b)])
                    P.add("act", lambda h, b=b, pt=pt: h.activation(
                        out=pt.ap, in_=ps[b][:, :].rearrange("p (a b) -> p a b", b=128), func=AF.Exp, scale=0.125),
                          r=[PS(b)], w=pt.all)
                return kbs, pts

            def swa_B(tb, g, kbs, pts):
                bo_ = SB_PV[g % 2]

                def f(h, bo_=bo_, g=g, kbs=kbs, pts=pts, tb=tb):
                    for j in range(4):
                        for n_, kb in enumerate(kbs):
                            slot = tb + 1 if kb == "cur" else tb
                            ins = h.matmul(ps[bo_][:, j * 128:j * 128 + 65], pts[kb].ap[:, j, :],
                                           sv.ap[:, slot, g, 0:65], start=(n_ == 0), stop=(n_ == len(kbs) - 1))
                    return ins
                P.add("pe", f, r=sum([pts[kb].all for kb in kbs], []) + sv.ch(tb, 2), w=[PS(bo_)])
                po = ps[bo_][:, :].rearrange("p (a b) -> p a b", b=128)
                P.add("dve", lambda h, po=po, g=g: h.tensor_tensor(out=den4.ap[:, 4 * g:4 * g + 4], in0=po[:, :, 64],
                                                                   in1=esink.ap[:, 4 * g:4 * g + 4], op=ALU.add),
                      r=[PS(bo_)] + esink.all, w=den4.all)
                P.add("dve", lambda h, g=g: h.reciprocal(out=rec4.ap[:, 4 * g:4 * g + 4],
                                                         in_=den4.ap[:, 4 * g:4 * g + 4]),
                      r=den4.all, w=rec4.all)
                P.add("dve", lambda h, po=po, g=g: h.tensor_tensor(
                    out=o_tm.ap.rearrange("p (h d) -> p h d", d=64)[:, 4 * g:4 * g + 4, :], in0=po[:, :, 0:64],
                    in1=rec4.ap[:, 4 * g:4 * g + 4].unsqueeze(2).to_broadcast([128, 4, 64]), op=ALU.mult),
                      r=[PS(bo_)] + rec4.all, w=o_tm.all)

            def swa_C(tb):
                cs = slice(tb * 128, (tb + 1) * 128)
                for half in range(2):
                    b = SB_TR

                    def f(h, b=b, half=half):
                        for c in range(4):
                            k = half * 4 + c
                            ins = h.matmul(ps[b][:, c * 128:(c + 1) * 128], o_tm.ap[:, k * 128:(k + 1) * 128], ident,
                                           start=True, stop=True)
                        return ins
                    P.add("pe", f, r=o_tm.all + cb.all, w=[PS(b)])
                    P.add("dve", lambda h, b=b, half=half, cs=cs: h.tensor_tensor(
                        out=bact.ap[:, half * 4:half * 4 + 4, cs], in0=ps[b][:, :].rearrange("p (a b) -> p a b", b=128),
                        in1=zact.ap[:, half * 4:half * 4 + 4, cs], op=ALU.mult),
                          r=[PS(b)] + zact.ch(half * 4, 4), w=bact.ch(half * 4, 4))

            a0_ = swa_A(0, 0)
            fm_group(G_SZ, hT, lambda j, b: act_evac(zact, AF.Silu, G_SZ)(j, b, 0))
            fm_group(G_SZ + 1, hT, lambda j, b: act_evac(zact, AF.Silu, G_SZ + 1)(j, b, 4))
            prev_u = None
            nu = 0
            for tb in range(4):
                for g in range(4):
                    a_ = a0_ if (tb, g) == (0, 0) else swa_A(tb, g)
                    if prev_u is not None:
                        swa_B(*prev_u)
                        if prev_u[1] == 3:
                            swa_C(prev_u[0])
                    prev_u = (tb, g) + a_
                    nu += 1
                    if nu % 2 == 0:
                        next(gg1, None)
            swa_B(*prev_u)
            swa_C(3)
            P.add("pool", lambda h: h.tensor_copy(out=kprev.ap[0:64], in_=krot.ap[0:64, :, 384:512]),
                  r=krot.all, w=kprev.all)
            P.add("pool", lambda h: h.tensor_copy(out=kprev_hi.ap[64:128], in_=khi.ap[64:128, :, 384:512]),
                  r=khi.all, w=kprev_hi.all)
            P.add("pool", lambda h: h.tensor_copy(out=sv.ap[:, 0, :, 0:64], in_=sv.ap[:, 4, :, 0:64]),
                  r=sv.ch(4), w=sv.ch(0))

            for _ in gg1:
                pass
            branch_mm(1, G_BRB)

            if _STOP == 7:
                break
            if t + 1 < NT:
                norm_transpose(x_tm, 4, gmx, hT, 512, phase="a", prescale=((t + 1) % TPS != 0))
            fm_group(G_XQ, hT, lambda j, b: act_evac(qbuf, AF.Identity, G_XQ)(j, b, 0))
            fm_group(G_XQ + 1, hT, lambda j, b: act_evac(qbuf, AF.Identity, G_XQ + 1)(j, b, 4))
            fm_group(G_XZ, hT, lambda j, b: act_evac(zact, AF.Silu, G_XZ)(j, b, 0))
            fm_group(G_XZ + 1, hT, lambda j, b: act_evac(zact, AF.Silu, G_XZ + 1)(j, b, 4))
            gg2 = gates_gen(2)
            for xh in range(4):
                next(gg2, None)
                next(gg2, None)
                for mb in range(2):
                    b = nb()

                    def f(h, b=b, mb=mb, xh=xh):
                        for dc in range(2):
                            ins = h.matmul(ps[b][:, :], mkT.ap[:, xh * 2 + dc, mb * 128:(mb + 1) * 128],
                                           qbuf.ap[:, xh * 2 + dc, :], start=(dc == 0), stop=(dc == 1))
                        return ins
                    P.add("pe", f, r=mkT.ch(xh * 2, 2) + qbuf.ch(xh * 2, 2), w=[PS(b)])
                    P.add("act", lambda h, b=b, mb=mb: h.activation(out=Px[mb].ap, in_=ps[b][:, :], func=AF.Exp,
                                                                    scale=1.0 / 16.0),
                          r=[PS(b)], w=Px[mb].all)
                bd = nb()

                def f(h, bd=bd):
                    for mb in range(2):
                        ins = h.matmul(ps[bd][:, :], ones, Px[mb].ap, start=(mb == 0), stop=(mb == 1))
                    return ins
                P.add("pe", f, r=Px[0].all + Px[1].all + cb.all, w=[PS(bd)])
                P.add("act", lambda h, bd=bd: h.activation(out=recx.ap, in_=ps[bd][:, :], func=AF.Ln),
                      r=[PS(bd)], w=recx.all)
                P.add("act", lambda h: h.activation(out=recx.ap, in_=recx.ap, func=AF.Exp, scale=-1.0),
                      r=recx.all, w=recx.all)
                for dc in range(2):
                    c = xh * 2 + dc
                    b = nb()

                    def f(h, b=b, c=c):
                        for mb in range(2):
                            ins = h.matmul(ps[b][:, :], mv.ap[:, mb, c * 128:(c + 1) * 128], Px[mb].ap,
                                           start=(mb == 0), stop=(mb == 1))
                        return ins
                    P.add("pe", f, r=mv.all + Px[0].all + Px[1].all, w=[PS(b)])
                    P.add("pool", lambda h, c=c, dc=dc: h.tensor_tensor(out=wx[dc].ap, in0=recx.ap, in1=zact.ap[:, c, :],
                                                                        op=ALU.mult),
                          r=recx.all + zact.ch(c), w=wx[dc].all)
                    P.add("dve", lambda h, b=b, c=c, dc=dc: h.tensor_tensor(out=bact.ap[:, c, :], in0=ps[b][:, :],
                                                                            in1=wx[dc].ap, op=ALU.mult),
                          r=[PS(b)] + wx[dc].all, w=bact.ch(c))

            for _ in gg2:
                pass
            if t + 1 < NT:
                prep_tile(t + 1, stats_done=True)
            branch_mm(2, G_BRC)

            if _STOP == 8:
                break
            for tb in range(4):
                dma("sp", xres[tb].ap, x_d[sq_i, tok0 + tb * 128:tok0 + (tb + 1) * 128, :], xres[tb].all, [], xrsem[tb])
            for half in range(2):
                def ev(tb, b, half=half):
                    xr = xres[tb]
                    P.add("dve", lambda h, b=b, xr=xr: h.scalar_tensor_tensor(
                        out=xr.ap[:, half * 512:(half + 1) * 512], in0=ps[b][:, :], scalar=0.5,
                        in1=xr.ap[:, half * 512:(half + 1) * 512], op0=ALU.mult, op1=ALU.add),
                          r=[PS(b)] + xr.all, w=xr.all)
                tm_group(G_OUT + half, qbuf, ev)
            for tb in range(4):
                xr = xres[tb]
                P.add("act", lambda h, tb=tb, xr=xr: h.activation(out=junk.ap, in_=xr.ap, func=AF.Square,
                                                                  accum_out=ss.ap[:, 4 + tb:5 + tb]),
                      r=xr.all, w=junk.all + ss.all)
                P.add("act", lambda h, tb=tb: h.activation(out=lnss.ap[:, 4 + tb:5 + tb], in_=ss.ap[:, 4 + tb:5 + tb],
                                                           func=AF.Ln, bias=EPS, scale=1.0 / 1024.0),
                      r=ss.all, w=lnss.all)
                P.add("act", lambda h, tb=tb: h.activation(out=rstd_x.ap[:, 4 + tb:5 + tb],
                                                           in_=lnss.ap[:, 4 + tb:5 + tb], func=AF.Exp, scale=-0.5),
                      r=lnss.all, w=rstd_x.all)
                P.add("dve", lambda h, tb=tb, xr=xr: h.scalar_tensor_tensor(
                    out=xr.ap, in0=xr.ap, scalar=rstd_x.ap[:, 4 + tb:5 + tb], in1=gfin,
                    op0=ALU.mult, op1=ALU.mult), r=xr.all + rstd_x.all + pf.all, w=xr.all)
                dma("sp", out_d[sq_i, tok0 + tb * 128:tok0 + (tb + 1) * 128, :], xr.ap, [], xr.all, osem[tb])

        P.add("sp", None, w=xres[0].all + xres[1].all + xres[2].all + xres[3].all)
        assert _STOP or wstate["used"] == len(wseq)
        P.add("pe", None, r=wbuf[0].all + wbuf[1].all + wbuf[2].all)
        P.add("sp", None, r=x_tm.all + mem_tm.all + pf.all)
        P.emit(nc, engsem)
    return nc


def _host_tables():
    cbt = np.zeros((128, C_END), np.float32)
    p = np.arange(128)
    cbt[:, C_ID:C_ID + 128] = np.eye(128, dtype=np.float32)
    cbt[:, C_ONE:C_ONE + 128] = 1.0
    perm = np.where((p % 64) < 32, p + 32, p - 32)
    pm = np.zeros((128, 128), np.float32)
    pm[perm, p] = 1.0
    cbt[:, C_PERM:C_PERM + 128] = pm
    tri = (p[:, None] <= p[None, :]).astype(np.float32)
    cbt[:, C_TRI:C_TRI + 512] = np.tile(tri, (1, 4))
    cbt[:, C_NTRI:C_NTRI + 512] = np.tile(-30000.0 * tri, (1, 4))
    cbt[:, C_NEGC:C_NEGC + 512] = np.tile(-30000.0 * (1.0 - tri), (1, 4))
    inv = (10000.0 ** (-np.arange(32, dtype=np.float32) / 32)).astype(np.float32)
    ang = np.arange(S, dtype=np.float32)[None, :] * inv[p % 32][:, None]
    cbt[:, C_COS:C_COS + S] = np.cos(ang)
    sgn = np.where((p % 64) < 32, -1.0, 1.0).astype(np.float32)
    cbt[:, C_SIN:C_SIN + S] = np.sin(ang) * sgn[:, None]
    return cbt


def _col_order():
    r = np.arange
    sk = np.concatenate([np.concatenate([r(O_SK + g * 64, O_SK + (g + 1) * 64)] * 2) for g in range(4)])
    cols = [r(O_GQ, O_GQ + 512), r(O_GK, O_GK + 512), r(O_GZ, O_GZ + 1024), r(O_SQ, O_SQ + 1024), sk,
            r(O_SZ, O_SZ + 1024), r(O_XQ, O_XQ + 1024), r(O_XZ, O_XZ + 1024), r(O_GT, O_GT + 3072),
            r(O_GV, O_GV + 1024), r(O_SV, O_SV + 256), r(O_GLR, O_GLR + 16)]
    return np.concatenate(cols)


def kernel(x, mem, g_mix, g_mem, w_in, b_in, w_gla_gate_up, b_gla_gate, g_gla_norm, sinks,
           w_mem_kv, w_br_gla, w_br_swa, w_br_mem, w_out, g_final, _NT=2 * TPS, _cores=NCORES):
    f32 = np.float32
    x = np.asarray(x, f32)
    mem = np.asarray(mem, f32)
    co = _col_order()
    w_in0 = np.asarray(w_in, f32)[0]
    b_in0 = np.asarray(b_in, f32)[0]
    w_all = np.zeros((D, NG * 512), f32)
    w_all[:, :co.size] = w_in0[:, co]
    w_all[:, G_BRA * 512:G_BRA * 512 + 1024] = np.asarray(w_br_gla, f32)[0]
    w_all[:, G_BRB * 512:G_BRB * 512 + 1024] = np.asarray(w_br_swa, f32)[0]
    w_all[:, G_BRC * 512:G_BRC * 512 + 1024] = np.asarray(w_br_mem, f32)[0]
    w_all[:, G_OUT * 512:G_OUT * 512 + 1024] = np.asarray(w_out, f32)[0]
    w_all[:, G_MK * 512:G_MK * 512 + 2048] = np.asarray(w_mem_kv, f32)[0]
    b_all = np.zeros(22 * 512, f32)
    b_all[:co.size] = b_in0[co]
    pf = np.zeros((128, PF_END), f32)
    pf[:, PF_BIAS:PF_BIAS + 76] = b_all[:76 * 128].reshape(76, 128).T
    pf[:, PF_GLA:PF_GLA + 2] = np.asarray(g_gla_norm, f32)[0].reshape(2, 128).T
    pf[:, PF_SINK:PF_SINK + 16] = np.broadcast_to(np.asarray(sinks, f32)[0][None, :], (128, 16))
    pf[0:16, PF_GLR] = b_all[21 * 512 + 256:21 * 512 + 272]
    pf[:, PF_GMX:PF_GMX + 1024] = np.repeat(np.asarray(g_mix, f32)[0].reshape(8, 128).T[:, :, None], 128, 2).reshape(128, 1024)
    pf[:, PF_GMEM:PF_GMEM + 1024] = np.repeat(np.asarray(g_mem, f32)[0].reshape(8, 128).T[:, :, None], 128, 2).reshape(128, 1024)
    pf[:, PF_GFIN:PF_GFIN + 1024] = np.broadcast_to(np.asarray(g_final, f32)[None, :], (128, 1024))
    brow = np.ascontiguousarray(b_all[19 * 512:22 * 512][None, :])
    wup = np.concatenate([np.asarray(w_gla_gate_up, f32)[0], np.asarray(b_gla_gate, f32)[0][None, :]], 0)
    cbt = _host_tables()

    nc = build_nc(_NT)
    in_maps = []
    for c in range(_cores):
        in_maps.append({"x": np.ascontiguousarray(x[2 * c:2 * c + 2]), "mem": np.ascontiguousarray(mem[2 * c:2 * c + 2]),
                        "w_all": w_all, "cb": cbt, "pf": pf, "brow": brow, "wup": np.ascontiguousarray(wup)})
    res = run_bass_kernel_spmd(nc, in_maps, core_ids=list(range(_cores)))
    return np.concatenate([res.results[c]["out"] for c in range(_cores)], axis=0).astype(np.float32)
```
